# Optimizing a Trainium2 kernel written in Bass

```python
import jax, jax.numpy as jnp
from jax import lax
import numpy as np

D_MODEL = 1024
BATCH = 8
SEQ = 2048
DEPTH = 4

GRID_W = 64
CTX_LEN = 256
HEAD_DIM = 64
N_Q_HEADS = 12
N_KV_HEADS = 4
Q_PER_KV = N_Q_HEADS // N_KV_HEADS
WINDOW = 128
BLOCK = 128
ROPE_BASE = 10000.0
N_FOURIER_GROUPS = 4
FOURIER_GROUP_DIM = 64
FOURIER_WIDTH = N_FOURIER_GROUPS * FOURIER_GROUP_DIM
ATTN_WIDTH = N_Q_HEADS * HEAD_DIM
KV_WIDTH = N_KV_HEADS * HEAD_DIM
Q_END = ATTN_WIDTH
K_END = Q_END + KV_WIDTH
V_END = K_END + KV_WIDTH
EVEN_IN_WIDTH = V_END + FOURIER_WIDTH
EVEN_MIX_WIDTH = ATTN_WIDTH + FOURIER_WIDTH
CONV_WIDTH = D_MODEL
CONV_K = 3
N_EXPERTS = 32
TOP_K = 4
D_EXPERT = D_MODEL
EXPERT_BLOCK = 128
SWIGLU_LIMIT = 7.0
SWIGLU_ALPHA = 1.702
LN_EPS = 1e-5
NEG_INF = -1e30
DEEPNORM_ALPHA = (2 * DEPTH) ** 0.25
DEEPNORM_BETA = (8 * DEPTH) ** -0.25
N_EVEN = (DEPTH + 1) // 2
N_ODD = DEPTH // 2

kernel_name = "hybrid_swa_fnet_shortconv_moe_dit"


def layer_norm(x, g, b):
    xf = x.astype(jnp.float32)
    mu = xf.mean(-1, keepdims=True)
    var = jnp.square(xf - mu).mean(-1, keepdims=True)
    return ((xf - mu) * lax.rsqrt(var + LN_EPS) * g.astype(jnp.float32) + b.astype(jnp.float32)).astype(x.dtype)


def axial_rope_tables(seq_len, dtype):
    rows = seq_len // GRID_W
    row = jnp.repeat(jnp.arange(rows), GRID_W).astype(jnp.float32)
    col = jnp.tile(jnp.arange(GRID_W), rows).astype(jnp.float32)
    n_freq = HEAD_DIM // 4
    inv = ROPE_BASE ** (-jnp.arange(n_freq, dtype=jnp.float32) / n_freq)
    ang = jnp.concatenate([row[:, None] * inv, col[:, None] * inv], -1)
    ang = jnp.concatenate([ang, ang], -1)
    return jnp.cos(ang).astype(dtype), jnp.sin(ang).astype(dtype)


def apply_rope(t, cos, sin):
    t1, t2 = jnp.split(t, 2, axis=-1)
    rot = jnp.concatenate([-t2, t1], -1)
    return t * cos[None, :, None, :] + rot * sin[None, :, None, :]


def sink_softmax(scores, sink):
    sk = sink.astype(jnp.float32).reshape(N_KV_HEADS, Q_PER_KV, 1, 1)
    m = sk
    for s in scores:
        m = jnp.maximum(m, s.max(axis=-1, keepdims=True))
    ex = [jnp.exp(s - m) for s in scores]
    denom = jnp.exp(sk - m)
    for e in ex:
        denom = denom + e.sum(axis=-1, keepdims=True)
    return [e / denom for e in ex]


def windowed_attention(q, k, v, kc, vc, sink):
    B, S = q.shape[0], q.shape[1]
    nb = S // BLOCK
    scale = HEAD_DIM ** -0.5
    qb = q.reshape(B, nb, BLOCK, N_KV_HEADS, Q_PER_KV, HEAD_DIM)

    def band(t):
        tp = jnp.pad(t, ((0, 0), (BLOCK, BLOCK), (0, 0), (0, 0))).reshape(B, nb + 2, BLOCK, N_KV_HEADS, HEAD_DIM)
        return jnp.concatenate([tp[:, :-2], tp[:, 1:-1], tp[:, 2:]], axis=2)

    kw, vw = band(k), band(v)
    s_loc = jnp.einsum('bnqkgd,bnjkd->bnkgqj', qb, kw).astype(jnp.float32) * scale
    qpos = jnp.arange(nb)[:, None, None] * BLOCK + jnp.arange(BLOCK)[None, :, None]
    kpos = jnp.arange(nb)[:, None, None] * BLOCK - BLOCK + jnp.arange(3 * BLOCK)[None, None, :]
    valid = (jnp.abs(qpos - kpos) <= WINDOW) & (kpos >= 0) & (kpos < S)
    s_loc = jnp.where(valid[None, :, None, None], s_loc, NEG_INF)
    s_ctx = jnp.einsum('bnqkgd,bckd->bnkgqc', qb, kc).astype(jnp.float32) * scale
    p_loc, p_ctx = sink_softmax([s_loc, s_ctx], sink)
    o = (jnp.einsum('bnkgqj,bnjkd->bnqkgd', p_loc.astype(vw.dtype), vw)
         + jnp.einsum('bnkgqc,bckd->bnqkgd', p_ctx.astype(vc.dtype), vc))
    return o.reshape(B, S, ATTN_WIDTH)


def context_attention(qc, kc, vc, sink):
    B, L = qc.shape[0], qc.shape[1]
    qg = qc.reshape(B, L, N_KV_HEADS, Q_PER_KV, HEAD_DIM)
    s = jnp.einsum('bqkgd,bckd->bkgqc', qg, kc).astype(jnp.float32) * (HEAD_DIM ** -0.5)
    (p,) = sink_softmax([s], sink)
    o = jnp.einsum('bkgqc,bckd->bqkgd', p.astype(vc.dtype), vc)
    return o.reshape(B, L, ATTN_WIDTH)


def fourier_mix(f):
    B, N = f.shape[0], f.shape[1]
    fg = f.astype(jnp.float32).reshape(B, N, N_FOURIER_GROUPS, FOURIER_GROUP_DIM)
    y = jnp.fft.fft2(fg, axes=(1, 3), norm='ortho').real
    return y.astype(f.dtype).reshape(B, N, FOURIER_WIDTH)


def even_mixer(u, uc, w_in, sink, w_out, cos, sin, ctx_out):
    B, S = u.shape[0], u.shape[1]
    L = uc.shape[1]
    h = u @ w_in
    q = apply_rope(h[..., :Q_END].reshape(B, S, N_Q_HEADS, HEAD_DIM), cos, sin)
    k = apply_rope(h[..., Q_END:K_END].reshape(B, S, N_KV_HEADS, HEAD_DIM), cos, sin)
    v = h[..., K_END:V_END].reshape(B, S, N_KV_HEADS, HEAD_DIM)
    if ctx_out:
        hc = uc @ w_in
        kvc = hc[..., Q_END:V_END]
    else:
        kvc = uc @ w_in[:, Q_END:V_END]
    kc = kvc[..., :KV_WIDTH].reshape(B, L, N_KV_HEADS, HEAD_DIM)
    vc = kvc[..., KV_WIDTH:].reshape(B, L, N_KV_HEADS, HEAD_DIM)
    y = jnp.concatenate([windowed_attention(q, k, v, kc, vc, sink), fourier_mix(h[..., V_END:])], -1) @ w_out
    if not ctx_out:
        return y, None
    qc = hc[..., :Q_END].reshape(B, L, N_Q_HEADS, HEAD_DIM)
    yc = jnp.concatenate([context_attention(qc, kc, vc, sink), fourier_mix(hc[..., V_END:])], -1) @ w_out
    return y, yc


def short_conv(h, w):
    return lax.conv_general_dilated(h, w[:, None, :].astype(h.dtype), window_strides=(1,), padding=((1, 1),),
                                    dimension_numbers=('NWC', 'WIO', 'NWC'), feature_group_count=CONV_WIDTH)


def gated_conv(u, w_in, conv_w, w_out):
    h = u @ w_in
    b_gate, c_gate, hh = jnp.split(h, 3, axis=-1)
    return (b_gate * short_conv(c_gate * hh, conv_w)) @ w_out


def moe(t, w_r, b_r, w_up, b_up, w_down, b_down):
    T, D = t.shape
    logits = (t @ w_r).astype(jnp.float32) + b_r.astype(jnp.float32)
    top_v, top_i = lax.top_k(logits, TOP_K)
    gates = jax.nn.softmax(top_v, axis=-1).astype(t.dtype)
    n = T * TOP_K
    flat_e = top_i.reshape(-1)
    order = jnp.argsort(flat_e)
    e_sorted = flat_e[order]
    tok_sorted = order // TOP_K
    sizes = jnp.bincount(flat_e, length=N_EXPERTS)
    padded = (sizes + EXPERT_BLOCK - 1) // EXPERT_BLOCK * EXPERT_BLOCK
    pad_ends = jnp.cumsum(padded)
    pad_starts = pad_ends - padded
    starts = jnp.cumsum(sizes) - sizes
    dest = pad_starts[e_sorted] + (jnp.arange(n) - starts[e_sorted])
    n_blk = (n + N_EXPERTS * (EXPERT_BLOCK - 1) + EXPERT_BLOCK - 1) // EXPERT_BLOCK
    xbuf = jnp.zeros((n_blk * EXPERT_BLOCK, D), t.dtype).at[dest].set(t[tok_sorted])
    blk_e = jnp.minimum(jnp.searchsorted(pad_ends, jnp.arange(n_blk) * EXPERT_BLOCK, side='right'), N_EXPERTS - 1)

    def expert_block(args):
        xb, e = args
        h = xb @ w_up[e] + b_up[e]
        glu = jnp.minimum(h[:, ::2], SWIGLU_LIMIT)
        lin = jnp.clip(h[:, 1::2], -SWIGLU_LIMIT, SWIGLU_LIMIT)
        a = glu * jax.nn.sigmoid(SWIGLU_ALPHA * glu) * (lin + 1)
        return a @ w_down[e] + b_down[e]

    out = lax.map(expert_block, (xbuf.reshape(n_blk, EXPERT_BLOCK, D), blk_e)).reshape(n_blk * EXPERT_BLOCK, D)
    ys = out[dest] * gates.reshape(-1)[order][:, None]
    return jax.ops.segment_sum(ys, tok_sorted, num_segments=T)


def setup_inputs(seed: int = 0) -> dict:
    key = jax.random.key(seed)
    ks = jax.random.split(key, 20)
    D = D_MODEL

    def nrm(k, shape, s):
        return jax.random.normal(k, shape, jnp.float32) * s

    return {
        "x": nrm(ks[0], (BATCH, SEQ, D), 1.0),
        "c": nrm(ks[1], (BATCH, D), 1.0),
        "ctx": nrm(ks[2], (BATCH, CTX_LEN, D), 1.0),
        "c_ctx": nrm(ks[3], (D,), 1.0),
        "w_mod": nrm(ks[4], (DEPTH, D, 6 * D), 0.5 * D ** -0.5),
        "b_mod": nrm(ks[5], (DEPTH, 6 * D), 0.02),
        "w_in_even": nrm(ks[6], (N_EVEN, D, EVEN_IN_WIDTH), D ** -0.5),
        "sink": nrm(ks[7], (N_EVEN, N_Q_HEADS), 0.5),
        "w_out_even": nrm(ks[8], (N_EVEN, EVEN_MIX_WIDTH, D), EVEN_MIX_WIDTH ** -0.5 * DEEPNORM_BETA),
        "w_in_odd": nrm(ks[9], (N_ODD, D, 3 * CONV_WIDTH), D ** -0.5),
        "conv_w": nrm(ks[10], (N_ODD, CONV_K, CONV_WIDTH), CONV_K ** -0.5),
        "w_out_odd": nrm(ks[11], (N_ODD, CONV_WIDTH, D), CONV_WIDTH ** -0.5 * DEEPNORM_BETA),
        "ln_g": 1.0 + nrm(ks[12], (DEPTH, 2, D), 0.02),
        "ln_b": nrm(ks[13], (DEPTH, 2, D), 0.02),
        "w_router": nrm(ks[14], (DEPTH, D, N_EXPERTS), D ** -0.5),
        "b_router": nrm(ks[15], (DEPTH, N_EXPERTS), 0.01),
        "w_up": nrm(ks[16], (DEPTH, N_EXPERTS, D, 2 * D_EXPERT), D ** -0.5),
        "b_up": nrm(ks[17], (DEPTH, N_EXPERTS, 2 * D_EXPERT), 0.02),
        "w_down": nrm(ks[18], (DEPTH, N_EXPERTS, D_EXPERT, D), D_EXPERT ** -0.5 * DEEPNORM_BETA),
        "b_down": nrm(ks[19], (DEPTH, N_EXPERTS, D), 0.02),
    }


def reference(x, c, ctx, c_ctx, w_mod, b_mod, w_in_even, sink, w_out_even, w_in_odd, conv_w, w_out_odd,
              ln_g, ln_b, w_router, b_router, w_up, b_up, w_down, b_down):
    B, S, D = x.shape
    L = ctx.shape[1]
    cos, sin = axial_rope_tables(S, x.dtype)
    silu_c = jax.nn.silu(c)
    silu_cc = jax.nn.silu(c_ctx)
    xc = ctx
    for l in range(DEPTH):
        even = l % 2 == 0
        j = l // 2
        ctx_after = any(m % 2 == 0 for m in range(l + 1, DEPTH))
        sh1, sc1, g1, sh2, sc2, g2 = jnp.split((silu_c @ w_mod[l] + b_mod[l])[:, None, :], 6, axis=-1)
        sh1c, sc1c, g1c, sh2c, sc2c, g2c = jnp.split(silu_cc @ w_mod[l] + b_mod[l], 6, axis=-1)
        u = x * (1 + sc1) + sh1
        if even:
            uc = xc * (1 + sc1c) + sh1c
            y, yc = even_mixer(u, uc, w_in_even[j], sink[j], w_out_even[j], cos, sin, ctx_after)
        else:
            y = gated_conv(u, w_in_odd[j], conv_w[j], w_out_odd[j])
            if ctx_after:
                uc = xc * (1 + sc1c) + sh1c
                yc = gated_conv(uc, w_in_odd[j], conv_w[j], w_out_odd[j])
        x = layer_norm(DEEPNORM_ALPHA * x + g1 * y, ln_g[l, 0], ln_b[l, 0])
        v = x * (1 + sc2) + sh2
        if ctx_after:
            xc = layer_norm(DEEPNORM_ALPHA * xc + g1c * yc, ln_g[l, 0], ln_b[l, 0])
            vc = xc * (1 + sc2c) + sh2c
            f_all = moe(jnp.concatenate([v.reshape(B * S, D), vc.reshape(B * L, D)], 0),
                        w_router[l], b_router[l], w_up[l], b_up[l], w_down[l], b_down[l])
            f = f_all[:B * S].reshape(B, S, D)
            fc = f_all[B * S:].reshape(B, L, D)
            xc = layer_norm(DEEPNORM_ALPHA * xc + g2c * fc, ln_g[l, 1], ln_b[l, 1])
        else:
            f = moe(v.reshape(B * S, D), w_router[l], b_router[l], w_up[l], b_up[l], w_down[l], b_down[l]).reshape(B, S, D)
        x = layer_norm(DEEPNORM_ALPHA * x + g2 * f, ln_g[l, 1], ln_b[l, 1])
    return x
```

```python
import numpy as np
import concourse.bass as bass
import concourse.mybir as mybir
from concourse.bass_utils import run_bass_kernel_spmd

F32, BF16 = mybir.dt.float32, mybir.dt.bfloat16
ALU = mybir.AluOpType
AF = mybir.ActivationFunctionType
AX = mybir.AxisListType

D, S, L, T = 1024, 2048, 256, 2304
NE = 32
ALPHA = 8 ** 0.25
LN_EPS = 1e-5
TT_LAT = [(0, 512), (512, 512), (1024, 512), (1536, 512)]
TT_CTX = [(2048, 256)]
UW = 3072
PAIRS = [(0, 3), (1, 4), (2, 5), (6, 9), (7, 10), (8, 11)]
ENGS = ("sp", "act", "dve", "pool", "pe")
MOE_DBG = [9]
RT_STEP = [99]
RT_NB = [99]


class Prog:
    def __init__(self, nc):
        self.nc = nc
        self.ops = {e: [] for e in ENGS}
        self.last_w = {}
        self.readers = {}
        self.waited = {e: {} for e in ENGS}
        self.dma_cnt = {}
        self.signal = {e: set() for e in ENGS}
        self.small = {e: set() for e in ENGS}
        self.default_small = False

    def add(self, eng, fn, reads=(), writes=(), dma=None, inc=16, sync_same=False, small=None):
        deps = []
        for t in reads:
            d = self.last_w.get(t)
            if d is not None:
                deps.append(d)
        for t in writes:
            d = self.last_w.get(t)
            if d is not None:
                deps.append(d)
            deps.extend(self.readers.get(t, ()))
        idx = len(self.ops[eng])
        if dma is not None:
            c = self.dma_cnt.get(dma, 0) + inc
            self.dma_cnt[dma] = c
            me = ("dma", dma, c)
        else:
            me = ("eng", eng, idx)
        wd = self.waited[eng]
        best = {}
        for d in deps:
            if d[0] == "eng":
                if d[1] == eng and dma is None and not sync_same and d[2] not in self.small[eng]:
                    continue
                key = ("eng", d[1])
            else:
                key = ("dma", d[1])
            val = d[2]
            if wd.get(key, -1) >= val:
                continue
            wd[key] = val
            if best.get(key, -1) < val:
                best[key] = val
            if d[0] == "eng":
                self.signal[d[1]].add(val)
        self.ops[eng].append((fn, list(best.items()), (dma, inc) if dma is not None else None))
        if (self.default_small if small is None else small) and dma is None:
            self.small[eng].add(idx)
        for t in reads:
            self.readers.setdefault(t, []).append(me)
        for t in writes:
            self.last_w[t] = me
            self.readers[t] = []
        return me

    def coalesce(self, sem):
        tot = self.dma_cnt.get(sem, 0)
        for k in list(self.last_w):
            d = self.last_w[k]
            if d[0] == "dma" and d[1] == sem:
                self.last_w[k] = ("dma", sem, tot)

    def barrier(self):
        lasts = [("eng", e, len(self.ops[e]) - 1) for e in ENGS if self.ops[e]]
        for e in ENGS:
            wd = self.waited[e]
            best = {}
            for d in lasts:
                if d[1] == e:
                    continue
                key = ("eng", d[1])
                if wd.get(key, -1) >= d[2]:
                    continue
                wd[key] = d[2]
                best[key] = d[2]
                self.signal[d[1]].add(d[2])
            if best:
                self.ops[e].append((None, list(best.items()), None))

    def emit(self, sems, dma_sems):
        nc = self.nc
        cum = {}
        for e in ENGS:
            cum[e] = {idx: i + 1 for i, idx in enumerate(sorted(self.signal[e]))}

        def run(e, obj):
            sig = cum[e]
            for idx, (fn, waits, dma) in enumerate(self.ops[e]):
                for (kind, key), val in waits:
                    if kind == "eng":
                        obj.wait_ge(sems[key], cum[key][val])
                    else:
                        obj.wait_ge(dma_sems[key], val)
                if fn is None:
                    if idx in sig:
                        obj.nop().then_inc(sems[e], 1)
                    continue
                inst = fn(obj)
                if dma is not None:
                    inst.then_inc(dma_sems[dma[0]], dma[1])
                    if idx in sig:
                        obj.nop().then_inc(sems[e], 1)
                elif idx in sig:
                    inst.then_inc(sems[e], 1)

        with nc.Block() as block:
            @block.sync
            def _(o):
                run("sp", o)

            @block.scalar
            def _(o):
                run("act", o)

            @block.vector
            def _(o):
                run("dve", o)

            @block.gpsimd
            def _(o):
                run("pool", o)

            @block.tensor
            def _(o):
                run("pe", o)


def unit_plan(layers):
    groups = [("misc", 6), ("tab", 4), ("mod", 8)]
    for l in layers:
        for q in range(4):
            groups.append((("moe", l, q), 8))
    offs = []
    o = 0
    for _, n in groups:
        offs.append(o)
        o += n
    return groups, offs, o


def build(layers=(0, 1, 2, 3), stop=None, n_experts=NE, single=False):
    nc = bass.Bass("TRN2", target_bir_lowering=False)
    groups, goffs, NU = unit_plan(layers)
    gidx = {g[0]: i for i, g in enumerate(groups)}

    def din(name, shape, dt=F32):
        return nc.dram_tensor(name, list(shape), dt, kind="ExternalInput")

    xT_d = din("xT", [128, 8, T])
    wsh_d = din("wsh", [NU * 128, UW])
    cvec_d = din("cvec", [128, 8, 2])
    bmod_d = din("bmod", [128, 4, 48])
    lng_d = din("lng", [128, 4, 2, 8])
    lnb_d = din("lnb", [128, 4, 2, 8])
    wr_d = din("wr", [128, 4, 8, 32])
    br_d = din("br", [128, 4, 32])
    bup_d = din("bup", [128, 4, 32, 16])
    bd2_d = din("bd2", [64, 4, 1024])
    sink_d = din("sinkb", [128, 2, 12])
    convw_d = din("convw", [128, 2, 3, 8])
    rope_d = din("rope", [128, 2, S])
    cs64_d = din("cs64", [128, 256])
    tabc_d = din("tabc", [128, 2, 2, 256])
    mask_d = din("maskd", [128, 2, 384])
    ident_d = din("ident", [128, 128])
    selc_d = din("selc", [64, 32])
    y_d = nc.dram_tensor("y", [128, 8, T], F32, kind="ExternalOutput")
    wb_d = nc.dram_tensor("wb", [NU * 128, UW], BF16)
    wg_d = [nc.dram_tensor("wg%d" % i, [8 * n * 128, UW], BF16) for i, (_, n) in enumerate(groups)]

    def wgv(gi):
        n = groups[gi][1]
        return wg_d[gi].ap().rearrange("(r u p) c -> r u p c", r=8, u=n)

    A = nc.alloc_sbuf_tensor
    XT = A("XT", [128, 8, T], F32)
    UT = A("UT", [128, 8, T], BF16)
    AT = A("AT", [128, 8 * T], BF16)
    RING = A("RING", [128, 8192], BF16)
    WD = A("WD", [128, 8192], BF16)
    SCR = A("SCR", [128, 9216], BF16)
    MODS = A("MODS", [128, 4, 48, 2], F32)
    NCS = 80
    CS = A("CS", [128, NCS, 8], F32)
    LNG = A("LNG", [128, 4, 2, 8], F32)
    LNB = A("LNB", [128, 4, 2, 8], F32)
    BUP = A("BUP", [128, 32, 16], F32)
    WR = A("WR", [128, 8, 32], F32)
    BR = A("BR", [128, 32], F32)
    IDENT = A("IDENT", [128, 128], F32)
    ONESF = A("ONESF", [128, 128], F32)
    ONESB = A("ONESB", [128, 128], BF16)
    SELC = A("SELC", [64, 32], BF16)
    SEXP = A("SEXP", [128, 2, 12], F32)
    CW = A("CW", [128, 2, 3, 8], F32)
    CVEC = A("CVEC", [128, 8, 2], F32)
    SCB = A("SCB", [128, 8, 2], BF16)
    CS64 = A("CS64", [128, 256], BF16)
    MASK = A("MASK", [128, 768], BF16)
    GHL = SCR[0:64, 6144:6144 + T]
    TABC = SCR[:, 4608:4608 + 1024]
    BM = SCR[:, 0:384].bitcast(F32).rearrange("p (l o) -> p l o", o=48)
    SMALLT = A("SMALLT", [128, 256], F32)
    SELE = A("SELE", [64, 256], BF16)
    GHBT = A("GHBT", [128, 32], BF16)
    SMALL = SMALLT[:]
    PS = [nc.alloc_psum_tensor("ps%d" % i, [128, 512], F32) for i in range(8)]

    p = Prog(nc)
    cs_names = {}

    def csi(name):
        if name not in cs_names:
            assert len(cs_names) < NCS, "CS overflow"
            cs_names[name] = len(cs_names)
        return cs_names[name]

    def csap(name, kc):
        i = csi(name)
        return CS[:, i, kc:kc + 1]

    def mm(out, lhsT, rhs, start, stop, reads, writes):
        p.add("pe", lambda e: e.matmul(out, lhsT, rhs, start=start, stop=stop), reads, writes)

    def sm(ap):
        return ap.free_size() < 256

    def act(out, in_, func, reads, writes, bias=0.0, scale=1.0):
        p.add("act", lambda e: e.activation(out=out, in_=in_, func=func, bias=bias, scale=scale), reads, writes,
              small=sm(out))

    def ts(out, in0, s1, s2, op0, op1, reads, writes, eng="dve"):
        if op1 is None:
            p.add(eng, lambda e: e.tensor_scalar(out=out, in0=in0, scalar1=s1, scalar2=None, op0=op0), reads, writes,
                  small=sm(out))
        else:
            p.add(eng, lambda e: e.tensor_scalar(out=out, in0=in0, scalar1=s1, scalar2=s2, op0=op0, op1=op1), reads, writes,
                  small=sm(out))

    def tt(out, in0, in1, op, reads, writes, eng="dve"):
        p.add(eng, lambda e: e.tensor_tensor(out=out, in0=in0, in1=in1, op=op), reads, writes, small=sm(out))

    def stt(out, in0, scalar, in1, op0, op1, reads, writes, eng="dve"):
        p.add(eng, lambda e: e.scalar_tensor_tensor(out=out, in0=in0, scalar=scalar, in1=in1, op0=op0, op1=op1),
              reads, writes, small=sm(out))

    def cp(out, in_, reads, writes, eng="dve"):
        p.add(eng, lambda e: e.tensor_copy(out=out, in_=in_), reads, writes, small=sm(out))

    def dma(q, out, in_, reads, writes, sem):
        p.add(q, lambda e: e.dma_start(out=out, in_=in_), reads, writes, dma=sem)

    def scr_f32(off_bytes, ncols):
        o = off_bytes // 2
        return SCR[:, o:o + 2 * ncols].bitcast(F32)

    def scr_bf(off_bytes, ncols):
        o = off_bytes // 2
        return SCR[:, o:o + ncols]

    for nm, t_sb, t_d in (("SELC", SELC[:], selc_d.ap()), ("CS64", CS64[:], cs64_d.ap()),
                          ("TABC", TABC, tabc_d.ap().rearrange("p a b c -> p (a b c)")),
                          ("MASK", MASK[:], mask_d.ap().rearrange("p a c -> p (a c)"))):
        dma("pool", t_sb, t_d, [], [("c", nm)], "cst")
    p.coalesce("cst")
    for gi, (gname, n) in enumerate([] if single else groups):
        r0, r1 = goffs[gi] * 128, (goffs[gi] + n) * 128
        p.add("pool", (lambda a, b: (lambda e: e.dma_start(out=wb_d.ap()[a:b, :], in_=wsh_d.ap()[a:b, :])))(r0, r1),
              writes=[("wb", gi)], dma="cast")
        p.add("pool", (lambda a, b, g: (lambda e: e.collective_compute(
            "AllGather", ALU.bypass, replica_groups=[list(range(8))],
            ins=[wb_d.ap()[a:b, :]], outs=[wg_d[g].ap()])))(r0, r1, gi),
            reads=[("wb", gi)], writes=[("wg", gi)], dma="cc", inc=1)

    for kc in range(8):
        dma("sp", XT[:, kc, :], xT_d.ap()[:, kc, :], [], [("XT", kc, t0) for t0, _ in TT_LAT + TT_CTX], "ld")
    for nm, t_sb, t_d in (("CVEC", CVEC[:], cvec_d.ap()), ("BM", BM, bmod_d.ap()), ("LNG", LNG[:], lng_d.ap()),
                          ("LNB", LNB[:], lnb_d.ap()), ("SEXP", SEXP[:], sink_d.ap()), ("CW", CW[:], convw_d.ap()),
                          ("IDENT", IDENT[:], ident_d.ap())):
        dma("sp", t_sb, t_d, [], [("c", nm)], "ld")
    p.coalesce("ld")

    p.add("dve", lambda e: e.memset(ONESF[:], 1.0 / D), writes=[("c", "ONESF")])
    p.add("dve", lambda e: e.memset(ONESB[:], 1.0), writes=[("c", "ONESB")])
    act(SEXP[:], SEXP[:], AF.Exp, [("c", "SEXP")], [("c", "SEXP")])
    act(SMALL[:, 0:16], CVEC[:].rearrange("p a b -> p (a b)"), AF.Sigmoid, [("c", "CVEC")], [("small", 0)])
    tt(SCB[:].rearrange("p a b -> p (a b)"), SMALL[:, 0:16], CVEC[:].rearrange("p a b -> p (a b)"), ALU.mult,
       [("small", 0), ("c", "CVEC")], [("c", "SCB")])

    gm = gidx["mod"]
    if single:
        p.add("dve", lambda e: e.memset(MODS[:], 0.5), writes=[("c", "MODS")])
    for l in ([] if single else range(4)):
        for pc in range(8):
            half, c0 = divmod(pc * 768, UW)
            buf = AT[:, (pc % 2) * 6144:(pc % 2 + 1) * 6144].rearrange("p (k c) -> p k c", c=768)
            dma("sp", buf, wgv(gm)[:, l * 2 + half, :, c0:c0 + 768].rearrange("k p c -> p k c"),
                [("wg", gm)], [("modw", pc % 2)], "mw%d" % (pc % 2))
            for cc in range(6):
                oc = pc * 6 + cc
                for kc in range(8):
                    mm(PS[0][:, oc * 2:oc * 2 + 2], buf[:, kc, cc * 128:(cc + 1) * 128], SCB[:, kc, :],
                       kc == 0, kc == 7, [("modw", pc % 2), ("c", "SCB")], [("ps", 0)])
        for s in range(2):
            tt(MODS[:, l, :, s], PS[0][:, 0:96].rearrange("p (o s) -> p o s", s=2)[:, :, s], BM[:, l, :], ALU.add,
               [("ps", 0), ("c", "BM")], [("c", "MODS")])

    def mod_ap(l, which, s):
        return MODS[:, l, which * 8:(which + 1) * 8, s]

    def cs_set(name, fn_emit):
        i = csi(name)
        fn_emit(CS[:, i, :])
        return i

    for l in range(4):
        for s in range(2):
            sfx = "%d_%d" % (l, s)
            ts(CS[:, csi("o1" + sfx), :], mod_ap(l, 1, s), 1.0, None, ALU.add, None, [("c", "MODS")], [("cs", "o1" + sfx)])
            ts(CS[:, csi("o2" + sfx), :], mod_ap(l, 4, s), 1.0, None, ALU.add, None, [("c", "MODS")], [("cs", "o2" + sfx)])
            ts(CS[:, csi("vsc" + sfx), :], CS[:, csi("o2" + sfx), :], 1.0 / ALPHA, None, ALU.mult, None,
               [("cs", "o2" + sfx)], [("cs", "vsc" + sfx)])
            tt(CS[:, csi("a2v" + sfx), :], LNG[:, l, 0, :], CS[:, csi("o2" + sfx), :], ALU.mult,
               [("c", "LNG"), ("cs", "o2" + sfx)], [("cs", "a2v" + sfx)])
            tt(CS[:, csi("b2v" + sfx), :], LNB[:, l, 0, :], CS[:, csi("o2" + sfx), :], ALU.mult,
               [("c", "LNB"), ("cs", "o2" + sfx)], [("cs", "b2v" + sfx)])
            tt(CS[:, csi("b2v" + sfx), :], CS[:, csi("b2v" + sfx), :], mod_ap(l, 3, s), ALU.add,
               [("cs", "b2v" + sfx), ("c", "MODS")], [("cs", "b2v" + sfx)])
            if l + 1 < 4:
                nsfx = "%d_%d" % (l + 1, s)
                ts(CS[:, csi("o1n" + sfx), :], mod_ap(l + 1, 1, s), 1.0, None, ALU.add, None, [("c", "MODS")],
                   [("cs", "o1n" + sfx)])
                tt(CS[:, csi("a2u" + sfx), :], LNG[:, l, 1, :], CS[:, csi("o1n" + sfx), :], ALU.mult,
                   [("c", "LNG"), ("cs", "o1n" + sfx)], [("cs", "a2u" + sfx)])
                tt(CS[:, csi("b2u" + sfx), :], LNB[:, l, 1, :], CS[:, csi("o1n" + sfx), :], ALU.mult,
                   [("c", "LNB"), ("cs", "o1n" + sfx)], [("cs", "b2u" + sfx)])
                tt(CS[:, csi("b2u" + sfx), :], CS[:, csi("b2u" + sfx), :], mod_ap(l + 1, 0, s), ALU.add,
                   [("cs", "b2u" + sfx), ("c", "MODS")], [("cs", "b2u" + sfx)])
        for k in range(2):
            nm = "%d_%d" % (l, k)
            ts(CS[:, csi("A1" + nm), :], LNG[:, l, k, :], ALPHA, None, ALU.mult, None, [("c", "LNG")], [("cs", "A1" + nm)])
            ts(CS[:, csi("B1" + nm), :], LNB[:, l, k, :], ALPHA, None, ALU.mult, None, [("c", "LNB")], [("cs", "B1" + nm)])

    def tiles_for(ctx_on):
        return TT_LAT + (TT_CTX if ctx_on else [])

    def xt_tok(kc, t0):
        return ("XT", kc, t0)

    def ut_tok(kc, t0):
        return ("UT", kc, t0)

    def stream_of(t0):
        return 1 if t0 >= S else 0

    def initial_u():
        for kc in range(8):
            for (t0, n) in TT_LAT + TT_CTX:
                s = stream_of(t0)
                sfx = "0_%d" % s
                act(UT[:, kc, t0:t0 + n], XT[:, kc, t0:t0 + n], AF.Identity,
                    [xt_tok(kc, t0), ("cs", "o1" + sfx), ("c", "MODS")], [ut_tok(kc, t0)],
                    bias=MODS[:, 0, kc:kc + 1, s], scale=csap("o1" + sfx, kc))
                ts(XT[:, kc, t0:t0 + n], XT[:, kc, t0:t0 + n], ALPHA, None, ALU.mult, None,
                   [xt_tok(kc, t0)], [xt_tok(kc, t0)])

    def layer_norm(l, k, ctx_on, final=False):
        SQ = [scr_f32(0, 512), scr_f32(2048, 512)]
        MEAN = scr_f32(4096, 512)
        VAR = scr_f32(6144, 512)
        TB = [scr_f32(8192, 512), scr_f32(10240, 512)]
        nm = "%d_%d" % (l, k)
        cnt = 0
        for (t0, n) in tiles_for(ctx_on):
            s = stream_of(t0)
            sfx = "%d_%d" % (l, s)
            for kc in range(8):
                sq = SQ[kc % 2]
                act(sq[:, 0:n], XT[:, kc, t0:t0 + n], AF.Square, [xt_tok(kc, t0)], [("sq", kc % 2)])
                mm(PS[6][:, 0:n], ONESF[:], XT[:, kc, t0:t0 + n], kc == 0, kc == 7,
                   [xt_tok(kc, t0), ("c", "ONESF")], [("ps", 6)])
                mm(PS[7][:, 0:n], ONESF[:], sq[:, 0:n], kc == 0, kc == 7, [("sq", kc % 2)], [("ps", 7)])
            act(MEAN[:, 0:n], PS[6][:, 0:n], AF.Copy, [("ps", 6)], [("mean",)])
            act(VAR[:, 0:n], PS[6][:, 0:n], AF.Square, [("ps", 6)], [("var",)])
            tt(VAR[:, 0:n], PS[7][:, 0:n], VAR[:, 0:n], ALU.subtract, [("ps", 7), ("var",)], [("var",)])
            ts(VAR[:, 0:n], VAR[:, 0:n], LN_EPS, None, ALU.add, None, [("var",)], [("var",)])
            act(VAR[:, 0:n], VAR[:, 0:n], AF.Ln, [("var",)], [("var",)])
            act(VAR[:, 0:n], VAR[:, 0:n], AF.Exp, [("var",)], [("var",)], scale=-0.5)
            for kc in range(8):
                tb = TB[cnt % 2]
                cnt += 1
                tt(tb[:, 0:n], XT[:, kc, t0:t0 + n], MEAN[:, 0:n], ALU.subtract,
                   [xt_tok(kc, t0), ("mean",)], [("tb", cnt % 2)])
                tt(tb[:, 0:n], tb[:, 0:n], VAR[:, 0:n], ALU.mult, [("tb", cnt % 2), ("var",)], [("tb", cnt % 2)])
                if final:
                    act(XT[:, kc, t0:t0 + n], tb[:, 0:n], AF.Identity, [("tb", cnt % 2), ("c", "LNG"), ("c", "LNB")],
                        [xt_tok(kc, t0)], bias=LNB[:, l, k, kc:kc + 1], scale=LNG[:, l, k, kc:kc + 1])
                    continue
                act(XT[:, kc, t0:t0 + n], tb[:, 0:n], AF.Identity, [("tb", cnt % 2), ("cs", "A1" + nm), ("cs", "B1" + nm)],
                    [xt_tok(kc, t0)], bias=csap("B1" + nm, kc), scale=csap("A1" + nm, kc))
                if k == 0:
                    an, bn = "a2v" + sfx, "b2v" + sfx
                else:
                    an, bn = "a2u" + sfx, "b2u" + sfx
                act(UT[:, kc, t0:t0 + n], tb[:, 0:n], AF.Identity, [("tb", cnt % 2), ("cs", an), ("cs", bn)],
                    [ut_tok(kc, t0)], bias=csap(bn, kc), scale=csap(an, kc))

    def resid_from_psum(ps, l, which, oc, t0, n, ps_tok):
        s = stream_of(t0)
        stt(XT[:, oc, t0:t0 + n], ps[:, 0:n], MODS[:, l, which * 8 + oc:which * 8 + oc + 1, s], XT[:, oc, t0:t0 + n],
            ALU.mult, ALU.add, [ps_tok, xt_tok(oc, t0), ("c", "MODS")], [xt_tok(oc, t0)])

    ring_state = {"n": 0}

    def moe(l, ctx_on):
        tiles = tiles_for(ctx_on)
        nblk = 18 if ctx_on else 16
        li = layers.index(l)
        dma("sp", BUP[:], bup_d.ap()[:, l, :, :], [], [("BUP",)], "ml")
        dma("sp", WR[:], wr_d.ap()[:, l, :, :], [], [("WR",)], "ml")
        dma("sp", BR[:], br_d.ap()[:, l, :], [], [("BR",)], "ml")
        BD2 = AT[0:64, 9216:9216 + 2048].bitcast(F32)
        GT2F = AT[0:64, 0:2 * T].bitcast(F32)
        dma("sp", BD2, bd2_d.ap()[:, l, :], [], [("BD2",)], "ml")
        p.coalesce("ml")
        ts(BUP[:, :, 8:16], BUP[:, :, 8:16], 1.0, None, ALU.add, None, [("BUP",)], [("BUP",)])
        if MOE_DBG[0] < 2:
            return
        VF = scr_f32(0, 1024).rearrange("p (k t) -> p k t", t=128)
        LG = SMALL[:, 0:32]
        MX = SMALL[:, 32:40]
        NEGM = SMALL[:, 40:41]
        SSUM = SMALL[:, 41:42]
        MSK = SMALL[:, 64:96]
        EX = SMALL[:, 96:128]
        GH = SMALL[:, 128:192]
        GHB = GHBT[:]
        for nb in range(min(nblk, RT_NB[0])):
            t0 = nb * 128
            s = stream_of(t0)
            sfx = "%d_%d" % (l, s)
            tile0 = (t0 // 512) * 512 if t0 < S else S
            for kc in range(8):
                act(VF[:, kc, :], XT[:, kc, t0:t0 + 128], AF.Identity, [xt_tok(kc, tile0), ("cs", "vsc" + sfx), ("c", "MODS")],
                    [("vf", kc)], bias=MODS[:, l, 24 + kc:25 + kc, s], scale=csap("vsc" + sfx, kc))
                mm(PS[5][:, 0:32], VF[:, kc, :], WR[:, kc, :], kc == 0, kc == 7, [("vf", kc), ("WR",)], [("ps", 5)])
            if RT_STEP[0] < 1:
                continue
            tt(LG, PS[5][:, 0:32], BR[:], ALU.add, [("ps", 5), ("BR",)], [("sm",)])
            if RT_STEP[0] < 2:
                continue
            p.add("dve", lambda e: e.max(out=MX, in_=LG), [("sm",)], [("sm",)], small=True)
            ts(MSK, LG, MX[:, 3:4], None, ALU.is_ge, None, [("sm",)], [("sm",)])
            ts(NEGM, MX[:, 0:1], -1.0, None, ALU.mult, None, [("sm",)], [("sm",)])
            if RT_STEP[0] < 3:
                continue
            act(EX, LG, AF.Exp, [("sm",)], [("sm",)], bias=NEGM)
            tt(EX, EX, MSK, ALU.mult, [("sm",)], [("sm",)])
            p.add("dve", lambda e: e.tensor_reduce(out=SSUM, in_=EX, axis=AX.X, op=ALU.add), [("sm",)], [("sm",)], small=True)
            p.add("dve", lambda e: e.reciprocal(out=SSUM, in_=SSUM), [("sm",)], [("sm",)], small=True)
            ts(EX, EX, SSUM, None, ALU.mult, None, [("sm",)], [("sm",)])
            if RT_STEP[0] < 4:
                continue
            cp(GHB, EX, [("sm",)], [("ghb",)])
            if RT_STEP[0] == 41:
                continue
            cp(GH[:, 0:32], GHB, [("ghb",)], [("sm",)])
            if RT_STEP[0] == 42:
                continue
            tt(GH[:, 32:64], EX, GH[:, 0:32], ALU.subtract, [("sm",)], [("sm",)])
            if RT_STEP[0] < 5:
                continue
            mm(PS[4][0:64, (nb % 4) * 128:(nb % 4) * 128 + 128], GH, IDENT[:], True, True,
               [("sm",), ("c", "IDENT")], [("ps", 4)])
            if RT_STEP[0] != 51 and RT_STEP[0] != 53:
                cp(GT2F[:, t0:t0 + 128], PS[4][0:64, (nb % 4) * 128:(nb % 4) * 128 + 128], [("ps", 4)], [("gt2f", tile0)], eng="dve")
            if RT_STEP[0] != 51 and RT_STEP[0] != 52:
                act(GHL[:, t0:t0 + 128], GT2F[:, t0:t0 + 128], AF.Copy, [("gt2f", tile0)], [("ghl", tile0)])
        if MOE_DBG[0] < 3:
            return
        for (t0, n) in tiles:
            for oc in range(8):
                pb = PS[6 + oc % 2]
                mm(pb[:, 0:n], BD2[:, oc * 128:(oc + 1) * 128], GT2F[:, t0:t0 + n], True, True,
                   [("BD2",), ("gt2f", t0)], [("ps", 6 + oc % 2)])
                resid_from_psum(pb, l, 5, oc, t0, n, ("ps", 6 + oc % 2))
        if MOE_DBG[0] < 4:
            return
        GC = [scr_f32(0, 512)] * 2
        SG = [scr_f32(2048, 512)] * 2
        T1 = [scr_f32(4096, 512)] * 2
        GS = scr_f32(6144, 512)
        TMP = [scr_f32(8192, 512), scr_f32(10240, 512)]
        ATV = AT[:].rearrange("p (j t) -> p j t", t=T)
        WDV = WD[:].rearrange("p (j c) -> p j c", c=1024)
        it = 0
        for e in range(n_experts):
            gi = gidx[("moe", l, e // 8)]
            u = e % 8
            dma("sp", WDV, wgv(gi)[:, u, :, 2048:3072].rearrange("j p c -> p j c"), [("wg", gi)], [("WD",)], "wd")
            for j in range(8):
                sl = ring_state["n"] % 4
                ring_state["n"] += 1
                RS = RING[:, sl * 2048:(sl + 1) * 2048].rearrange("p (k c) -> p k c", c=256)
                dma("sp", RING[:, sl * 2048:(sl + 1) * 2048], wgv(gi)[j, u, :, 0:2048], [("wg", gi)], [("ring", sl)],
                    "rg%d" % sl)
                for (t0, n) in tiles:
                    b = it % 2
                    it += 1
                    pg, pl = PS[b], PS[2 + b]
                    for kc in range(8):
                        mm(pg[:, 0:n], RS[:, kc, 0:128], UT[:, kc, t0:t0 + n], kc == 0, kc == 7,
                           [("ring", sl), ut_tok(kc, t0)], [("ps", b)])
                    for kc in range(8):
                        mm(pl[:, 0:n], RS[:, kc, 128:256], UT[:, kc, t0:t0 + n], kc == 0, kc == 7,
                           [("ring", sl), ut_tok(kc, t0)], [("ps", 2 + b)])
                    ts(T1[b][:, 0:n], pl[:, 0:n], BUP[:, e, 8 + j:9 + j], -6.0, ALU.add, ALU.max,
                       [("ps", 2 + b), ("BUP",)], [("t1",)])
                    ts(GC[b][:, 0:n], pg[:, 0:n], BUP[:, e, j:j + 1], 7.0, ALU.add, ALU.min,
                       [("ps", b), ("BUP",)], [("gc",)])
                    act(SG[b][:, 0:n], GC[b][:, 0:n], AF.Sigmoid, [("gc",)], [("sg",)], scale=1.702)
                    tt(GC[b][:, 0:n], GC[b][:, 0:n], SG[b][:, 0:n], ALU.mult, [("gc",), ("sg",)], [("gc",)])
                    stt(ATV[:, j, t0:t0 + n], T1[b][:, 0:n], 8.0, GC[b][:, 0:n], ALU.min, ALU.mult,
                        [("t1",), ("gc",)], [("at", j, t0)])
            sele = SELE[:, (e % 2) * 128:(e % 2 + 1) * 128]
            cp(sele, SELC[:, e:e + 1].to_broadcast([64, 128]), [("c", "SELC")], [("sele", e % 2)])
            for (t0, n) in tiles:
                mm(PS[4][:, 0:n], sele, GHL[:, t0:t0 + n], True, True,
                   [("sele", e % 2), ("ghl", t0)], [("ps", 4)])
                act(GS[:, 0:n], PS[4][:, 0:n], AF.Copy, [("ps", 4)], [("gs",)])
                for oc in range(8):
                    b = oc % 2
                    pd = PS[6 + b]
                    for j in range(8):
                        mm(pd[:, 0:n], WDV[:, j, oc * 128:(oc + 1) * 128], ATV[:, j, t0:t0 + n], j == 0, j == 7,
                           [("WD",), ("at", j, t0)], [("ps", 6 + b)])
                    s = stream_of(t0)
                    stt(TMP[b][:, 0:n], pd[:, 0:n], MODS[:, l, 40 + oc:41 + oc, s], GS[:, 0:n], ALU.mult, ALU.mult,
                        [("ps", 6 + b), ("gs",), ("c", "MODS")], [("tmp", b)])
                    tt(XT[:, oc, t0:t0 + n], XT[:, oc, t0:t0 + n], TMP[b][:, 0:n], ALU.add,
                       [("tmp", b), xt_tok(oc, t0)], [xt_tok(oc, t0)])

    def odd_mixer(l, ctx_on):
        j = l // 2
        tiles = tiles_for(ctx_on)
        gmi = gidx["misc"]
        WDV = WD[:].rearrange("p (k c) -> p k c", c=1024)
        dma("sp", WDV, wgv(gmi)[:, 4 + j, :, 1024:2048].rearrange("k p c -> p k c"), [("wg", gmi)], [("WD",)], "wd")
        GTV = AT[:].rearrange("p (j t) -> p j t", t=T)
        Z = scr_f32(0, 2312)
        BG = scr_f32(9248, 512)
        CB = scr_f32(11296, 512)
        A1 = scr_f32(13344, 512)
        for col in (0, 2049, 2050, 2307):
            p.add("dve", (lambda c: (lambda e: e.memset(Z[:, c:c + 1], 0.0)))(col), writes=[("zpad",)])

        def zoff(t0):
            return 1 + t0 if t0 < S else 2051 + (t0 - S)

        BGALL = AT
        for fc in range(8):
            sl = fc % 2
            RS = RING[:, sl * 3072:(sl + 1) * 3072].rearrange("p (k c) -> p k c", c=384)
            for part in range(3):
                dma("sp", RS[:, :, part * 128:(part + 1) * 128],
                    wgv(gmi)[:, j, :, part * 1024 + fc * 128:part * 1024 + (fc + 1) * 128].rearrange("k p c -> p k c"),
                    [("wg", gmi)], [("ring", sl)], "rg%d" % sl)
            for ti, (t0, n) in enumerate(tiles):
                b = ti % 2
                for part, pb in ((1, PS[b]), (2, PS[2 + b])):
                    for kc in range(8):
                        mm(pb[:, 0:n], RS[:, kc, part * 128:(part + 1) * 128], UT[:, kc, t0:t0 + n], kc == 0, kc == 7,
                           [("ring", sl), ut_tok(kc, t0)], [("ps", (0 if part == 1 else 2) + b)])
                act(CB[:, 0:n], PS[b][:, 0:n], AF.Copy, [("ps", b)], [("cb",)])
                tt(Z[:, zoff(t0):zoff(t0) + n], PS[2 + b][:, 0:n], CB[:, 0:n], ALU.mult, [("ps", 2 + b), ("cb",)], [("z", t0)])
            for ti, (t0, n) in enumerate(tiles):
                b = ti % 2
                for kc in range(8):
                    mm(PS[4 + b][:, 0:n], RS[:, kc, 0:128], UT[:, kc, t0:t0 + n], kc == 0, kc == 7,
                       [("ring", sl), ut_tok(kc, t0)], [("ps", 4 + b)])
                zo = zoff(t0)
                zr = [("z", tt0) for tt0, _ in tiles] + [("zpad",)]
                ts(A1[:, 0:n], Z[:, zo:zo + n], CW[:, j, 1, fc:fc + 1], None, ALU.mult, None, zr + [("c", "CW")], [("a1",)])
                stt(A1[:, 0:n], Z[:, zo - 1:zo - 1 + n], CW[:, j, 0, fc:fc + 1], A1[:, 0:n], ALU.mult, ALU.add,
                    zr + [("a1",)], [("a1",)])
                stt(A1[:, 0:n], Z[:, zo + 1:zo + 1 + n], CW[:, j, 2, fc:fc + 1], A1[:, 0:n], ALU.mult, ALU.add,
                    zr + [("a1",)], [("a1",)])
                tt(GTV[:, fc, t0:t0 + n], PS[4 + b][:, 0:n], A1[:, 0:n], ALU.mult, [("ps", 4 + b), ("a1",)], [("gt", fc, t0)])
        p.barrier()
        for (t0, n) in tiles:
            for oc in range(8):
                b = oc % 2
                for fc in range(8):
                    mm(PS[6 + b][:, 0:n], WDV[:, fc, oc * 128:(oc + 1) * 128], GTV[:, fc, t0:t0 + n], fc == 0, fc == 7,
                       [("WD",), ("gt", fc, t0)], [("ps", 6 + b)])
                resid_from_psum(PS[6 + b], l, 2, oc, t0, n, ("ps", 6 + b))

    def even_mixer(l, ctx_out):
        j = l // 2
        gmi, gti = gidx["misc"], gidx["tab"]
        un = 2 + j
        FTv = SCR[:, 0:2 * T].rearrange("p (f t) -> p f t", t=T)
        Gv = AT[:, 0:9216].rearrange("p (n c) -> p n c", c=512)
        SETS = [AT[:, 9216:17408].rearrange("p (n c) -> p n c", c=512), WD[:, 0:8192].rearrange("p (n c) -> p n c", c=512)]
        RS0 = RING[:, 0:2048].rearrange("p (k c) -> p k c", c=256)
        tiles_f = tiles_for(ctx_out)
        dma("sp", RS0, wgv(gmi)[:, un, :, 2304:2560].rearrange("k p c -> p k c"), [("wg", gmi)], [("ring", 0)], "rg0")
        for ti, (t0, n) in enumerate(tiles_f):
            for fcn in range(2):
                b = (ti * 2 + fcn) % 2
                for kc in range(8):
                    mm(PS[b][:, 0:n], RS0[:, kc, fcn * 128:(fcn + 1) * 128], UT[:, kc, t0:t0 + n], kc == 0, kc == 7,
                       [("ring", 0), ut_tok(kc, t0)], [("ps", b)])
                act(FTv[:, fcn, t0:t0 + n], PS[b][:, 0:n], AF.Copy, [("ps", b)], [("ft", fcn, t0)])
        nblk_f = 18 if ctx_out else 16
        for nb in range(nblk_f):
            t0 = nb * 128
            tile0 = (t0 // 512) * 512 if t0 < S else S
            for fcn in range(2):
                b = (nb * 2 + fcn) % 2
                mm(PS[4 + b][:, 0:256], FTv[:, fcn, t0:t0 + 128], CS64[:], True, True,
                   [("ft", fcn, tile0), ("c", "CS64")], [("ps", 4 + b)])
                if b == 0:
                    cp(Gv[:, nb, fcn * 256:(fcn + 1) * 256], PS[4 + b][:, 0:256], [("ps", 4 + b)], [("g", nb)])
                else:
                    act(Gv[:, nb, fcn * 256:(fcn + 1) * 256], PS[4 + b][:, 0:256], AF.Copy, [("ps", 4 + b)], [("g", nb)])
        p.barrier()
        gall = [("g", nb) for nb in range(16)]
        for kt in range(8):
            k0 = kt * 256
            st = SETS[kt % 2]
            for which in range(2):
                for u2 in range(2):
                    dma("sp", st[:, :, which * 256:(which + 1) * 256].rearrange("p (r u) c -> p r u c", u=2)[:, :, u2, :],
                        wgv(gti)[:, 2 * which + u2, :, k0:k0 + 256].rearrange("r p c -> p r c"),
                        [("wg", gti)], [("tabset", kt % 2)], "tb%d" % (kt % 2))
            for fcn in range(2):
                b = (kt * 2 + fcn) % 2
                for nb in range(16):
                    mm(PS[b][:, 0:256], Gv[:, nb, fcn * 256:fcn * 256 + 128], st[:, nb, 0:256], nb == 0, False,
                       [("tabset", kt % 2)] + gall, [("ps", b)])
                    mm(PS[b][:, 0:256], Gv[:, nb, fcn * 256 + 128:fcn * 256 + 256], st[:, nb, 256:512], False, nb == 15,
                       [("tabset", kt % 2)] + gall, [("ps", b)])
                act(FTv[:, fcn, k0:k0 + 256], PS[b][:, 0:256], AF.Copy, [("ps", b)], [("ft", fcn, (k0 // 512) * 512)])
        if ctx_out:
            TABCv = TABC.rearrange("p (a b c) -> p a b c", a=2, b=2)
            for fcn in range(2):
                for nb in range(2):
                    mm(PS[2][:, 0:256], Gv[:, 16 + nb, fcn * 256:fcn * 256 + 128], TABCv[:, 0, nb, :], nb == 0, False,
                       [("c", "TABC"), ("g", 16 + nb)], [("ps", 2)])
                    mm(PS[2][:, 0:256], Gv[:, 16 + nb, fcn * 256 + 128:fcn * 256 + 256], TABCv[:, 1, nb, :], False, nb == 1,
                       [("c", "TABC"), ("g", 16 + nb)], [("ps", 2)])
                act(FTv[:, fcn, S:S + 256], PS[2][:, 0:256], AF.Copy, [("ps", 2)], [("ft", fcn, S)])
        p.barrier()
        ROPE = RING[:].bitcast(F32).rearrange("p (a t) -> p a t", t=S)
        dma("sp", ROPE, rope_d.ap(), [], [("rope",)], "rp")
        QTv = AT[:, 0:6 * T].rearrange("p (s t) -> p s t", t=T)
        KTv = AT[:, 6 * T:8 * T].rearrange("p (s t) -> p s t", t=T)
        Vv = SCR[:, 4608:9216].rearrange("p (n c) -> p n c", c=256)
        SL = [WD[:, 0:2048].rearrange("p (k c) -> p k c", c=256), WD[:, 2048:4096].rearrange("p (k c) -> p k c", c=256)]
        X1 = [WD[:, 4096:5120].bitcast(F32), WD[:, 6144:7168].bitcast(F32)]
        X2 = [WD[:, 5120:6144].bitcast(F32), WD[:, 7168:8192].bitcast(F32)]
        pieces = [("q", s) for s in range(6)] + [("k", g2) for g2 in range(2)]
        cnt = 0
        for pi, (kind, s) in enumerate(pieces):
            sl = pi % 2
            ca = (s * 128) if kind == "q" else (1536 + s * 128)
            cb_ = (768 + s * 128) if kind == "q" else (1792 + s * 128)
            for half, c0 in ((0, ca), (1, cb_)):
                dma("sp", SL[sl][:, :, half * 128:(half + 1) * 128],
                    wgv(gmi)[:, un, :, c0:c0 + 128].rearrange("k p c -> p k c"), [("wg", gmi)], [("wsl", sl)], "ws%d" % sl)
            dst = QTv if kind == "q" else KTv
            tk = "qt" if kind == "q" else "kt"
            tl = TT_LAT + (TT_CTX if (kind == "k" or ctx_out) else [])
            for (t0, n) in tl:
                b = cnt % 2
                cnt += 1
                for kc in range(8):
                    mm(PS[b][:, 0:n], SL[sl][:, kc, 0:128], UT[:, kc, t0:t0 + n], kc == 0, kc == 7,
                       [("wsl", sl), ut_tok(kc, t0)], [("ps", b)])
                if t0 >= S:
                    act(dst[:, s, t0:t0 + n], PS[b][:, 0:n], AF.Copy, [("ps", b)], [(tk, s, t0)])
                    continue
                for kc in range(8):
                    mm(PS[2 + b][:, 0:n], SL[sl][:, kc, 128:256], UT[:, kc, t0:t0 + n], kc == 0, kc == 7,
                       [("wsl", sl), ut_tok(kc, t0)], [("ps", 2 + b)])
                tt(X1[b][:, 0:n], PS[b][:, 0:n], ROPE[:, 0, t0:t0 + n], ALU.mult, [("ps", b), ("rope",)], [("x1", b)])
                tt(X2[b][:, 0:n], PS[2 + b][:, 0:n], ROPE[:, 1, t0:t0 + n], ALU.mult, [("ps", 2 + b), ("rope",)], [("x2", b)])
                tt(dst[:, s, t0:t0 + n], X1[b][:, 0:n], X2[b][:, 0:n], ALU.add, [("x1", b), ("x2", b)], [(tk, s, t0)])
        sl = len(pieces) % 2
        dma("sp", SL[sl], wgv(gmi)[:, un, :, 2048:2304].rearrange("k p c -> p k c"), [("wg", gmi)], [("wsl", sl)], "ws%d" % sl)
        for nb in range(18):
            b = nb % 2
            t0 = nb * 128
            tile0 = (t0 // 512) * 512 if t0 < S else S
            for kc in range(8):
                mm(PS[4 + b][:, 0:256], UT[:, kc, t0:t0 + 128], SL[sl][:, kc, :], kc == 0, kc == 7,
                   [("wsl", sl), ut_tok(kc, tile0)], [("ps", 4 + b)])
            if b == 0:
                cp(Vv[:, nb, :], PS[4 + b][:, 0:256], [("ps", 4 + b)], [("v", nb)])
            else:
                act(Vv[:, nb, :], PS[4 + b][:, 0:256], AF.Copy, [("ps", 4 + b)], [("v", nb)])
        p.barrier()
        PT = [WD[:, i * 384:(i + 1) * 384] for i in range(5)]
        DN = WD[:, 2048:2816].bitcast(F32)
        qblocks = list(range(16)) + ([16, 17] if ctx_out else [])
        cnt = 0
        for qb in qblocks:
            q0 = qb * 128
            qtile = (q0 // 512) * 512 if q0 < S else S
            for g in range(4):
                h0 = 64 * (g % 2)
                g2 = g // 2
                s0 = 3 * g2
                if qb < 16:
                    kbs = [kb for kb in (qb - 1, qb, qb + 1) if 0 <= kb <= 15] + [16, 17]
                else:
                    kbs = [16, 17]
                for idx, kb in enumerate(kbs):
                    k0 = kb * 128
                    ktile = (k0 // 512) * 512 if k0 < S else S
                    psS = PS[idx % 3]
                    for i in range(3):
                        mm(psS[:, i * 128:(i + 1) * 128], KTv[h0:h0 + 64, g2, k0:k0 + 128],
                           QTv[h0:h0 + 64, s0 + i, q0:q0 + 128], True, True,
                           [("kt", g2, ktile), ("qt", s0 + i, qtile)], [("ps", idx % 3)])
                    act(PT[idx], psS[:, 0:384], AF.Exp, [("ps", idx % 3)], [("pt", idx)], scale=0.125)
                    if qb < 16 and kb == qb - 1:
                        tt(PT[idx], PT[idx], MASK[:, 0:384], ALU.mult, [("pt", idx), ("c", "MASK")], [("pt", idx)])
                    elif qb < 16 and kb == qb + 1:
                        tt(PT[idx], PT[idx], MASK[:, 384:768], ALU.mult, [("pt", idx), ("c", "MASK")], [("pt", idx)])
                b = cnt % 2
                cnt += 1
                psO, psD = PS[3 + b], PS[5 + b]
                nk = len(kbs)
                for idx, kb in enumerate(kbs):
                    mm(psO[:, 0:384], Vv[:, kb, g2 * 128:(g2 + 1) * 128], PT[idx], idx == 0, idx == nk - 1,
                       [("v", kb), ("pt", idx)], [("ps", 3 + b)])
                for idx, kb in enumerate(kbs):
                    mm(psD[:, 0:384], ONESB[:], PT[idx], idx == 0, idx == nk - 1,
                       [("c", "ONESB"), ("pt", idx)], [("ps", 5 + b)])
                for i in range(3):
                    ts(DN[h0:h0 + 64, i * 128:(i + 1) * 128], psD[h0:h0 + 64, i * 128:(i + 1) * 128],
                       SEXP[h0:h0 + 64, j, 3 * g + i:3 * g + i + 1], None, ALU.add, None,
                       [("ps", 5 + b), ("c", "SEXP")], [("dn",)])
                p.add("dve", (lambda a: (lambda e: e.reciprocal(out=DN[a:a + 64, :], in_=DN[a:a + 64, :])))(h0),
                      [("dn",)], [("dn",)], small=True)
                tt(UT[h0:h0 + 64, s0:s0 + 3, q0:q0 + 128], psO[h0:h0 + 64, 0:384].rearrange("p (s q) -> p s q", q=128),
                   DN[h0:h0 + 64, :].rearrange("p (s q) -> p s q", q=128), ALU.mult,
                   [("ps", 3 + b), ("dn",)], [ut_tok(s0 + i, qtile) for i in range(3)])
        p.barrier()
        WOV = WD[:].rearrange("p (k c) -> p k c", c=1024)
        dma("sp", WOV, wgv(gmi)[:, 4 + j, :, 0:1024].rearrange("k p c -> p k c"), [("wg", gmi)], [("WD",)], "wd")
        for (t0, n) in tiles_f:
            for oc in range(8):
                b = oc % 2
                for c8 in range(8):
                    rhs = UT[:, c8, t0:t0 + n] if c8 < 6 else FTv[:, c8 - 6, t0:t0 + n]
                    tok = ut_tok(c8, t0) if c8 < 6 else ("ft", c8 - 6, t0)
                    mm(PS[6 + b][:, 0:n], WOV[:, c8, oc * 128:(oc + 1) * 128], rhs, c8 == 0, c8 == 7,
                       [("WD",), tok], [("ps", 6 + b)])
                resid_from_psum(PS[6 + b], l, 2, oc, t0, n, ("ps", 6 + b))

    p.barrier()
    if single:
        moe(0, True)
        p.barrier()
        layers = ()
    done = False
    if 0 in layers:
        initial_u()
    p.barrier()
    for l in layers:
        even = (l % 2 == 0)
        ctx_after = (l < 2)
        mix_ctx = ctx_after
        if stop == ("pre", l):
            break
        if even:
            even_mixer(l, mix_ctx)
        else:
            odd_mixer(l, mix_ctx)
        p.barrier()
        if stop == ("mix", l):
            break
        layer_norm(l, 0, ctx_after)
        p.barrier()
        if stop == ("ln1", l):
            break
        moe(l, ctx_after)
        p.barrier()
        if stop == ("moe", l):
            break
        layer_norm(l, 1, ctx_after, final=(l == 3))
        p.barrier()
    for kc in range(8):
        dma("sp", y_d.ap()[:, kc, :], XT[:, kc, :], [xt_tok(kc, t0) for t0, _ in TT_LAT + TT_CTX], [("y", kc)], "out")
    p.add("sp", None, reads=[("y", kc) for kc in range(8)])
    sems = {e: nc.alloc_semaphore("s_" + e) for e in ENGS}
    dsems = {k: nc.alloc_semaphore("d_" + str(k)) for k in p.dma_cnt}
    with nc.allow_low_precision("bf16 matmul operands, fp32 accumulation"):
        p.emit(sems, dsems)
    return nc


def _fm(v):
    v = np.asarray(v, np.float32)
    lead = v.shape[:-1]
    return np.ascontiguousarray(np.moveaxis(v.reshape(lead + (8, 128)), -1, 0))


def prep_inputs(inputs, layers=(0, 1, 2, 3)):
    f32 = np.float32
    x, c, ctx, c_ctx = (np.asarray(inputs[k], f32) for k in ("x", "c", "ctx", "c_ctx"))
    groups, goffs, NU = unit_plan(layers)
    w_in_even, w_out_even = np.asarray(inputs["w_in_even"], f32), np.asarray(inputs["w_out_even"], f32)
    w_in_odd, w_out_odd = np.asarray(inputs["w_in_odd"], f32), np.asarray(inputs["w_out_odd"], f32)
    w_mod, w_up, w_down = inputs["w_mod"], inputs["w_up"], inputs["w_down"]
    qcols, qswap = [], []
    for lo, hi in PAIRS:
        for h in (lo, hi):
            qcols += list(range(h * 64, h * 64 + 64))
            qswap += list(range(h * 64 + 32, h * 64 + 64)) + list(range(h * 64, h * 64 + 32))
    kcols = list(range(768, 1024))
    kswap = []
    for g in range(4):
        kswap += list(range(768 + g * 64 + 32, 768 + g * 64 + 64)) + list(range(768 + g * 64, 768 + g * 64 + 32))
    ecols = qcols + qswap + kcols + kswap + list(range(1024, 1536))
    orow = qcols + list(range(768, 1024))
    n = np.arange(S, dtype=np.float64)
    ang = 2 * np.pi * np.outer(n, n) / S
    sc = 1.0 / np.sqrt(S * 64.0)
    CN = (np.cos(ang) * sc).astype(f32)
    SN = (-np.sin(ang) * sc).astype(f32)
    wsh = np.zeros((8, NU, 128, UW), f32)
    for r in range(8):
        rows = slice(r * 128, (r + 1) * 128)
        for jj in range(2):
            wsh[r, jj] = w_in_odd[jj][rows, :]
            wsh[r, 2 + jj, :, :2560] = w_in_even[jj][rows][:, ecols]
            wsh[r, 4 + jj, :, :1024] = w_out_even[jj][orow][rows, :]
            wsh[r, 4 + jj, :, 1024:2048] = w_out_odd[jj][rows, :]
        for u in range(2):
            rr = slice(r * 256 + u * 128, r * 256 + (u + 1) * 128)
            wsh[r, 6 + u, :, :S] = CN[rr]
            wsh[r, 8 + u, :, :S] = SN[rr]
        for l in range(4):
            for half in range(2):
                wsh[r, 10 + l * 2 + half] = w_mod[l][rows, half * UW:(half + 1) * UW]
    for gi, (gname, nun) in enumerate(groups):
        if not isinstance(gname, tuple):
            continue
        _, l, q = gname
        for u in range(8):
            e = q * 8 + u
            wu = np.asarray(w_up[l, e], f32)
            wd = np.asarray(w_down[l, e], f32)
            for r in range(8):
                jsl = slice(r * 128, (r + 1) * 128)
                piece = np.empty((8, 128, 256), f32)
                piece[:, :, :128] = wu[:, 0::2][:, jsl].reshape(8, 128, 128)
                piece[:, :, 128:] = wu[:, 1::2][:, jsl].reshape(8, 128, 128)
                wsh[r, goffs[gi] + u, :, :2048] = piece.transpose(1, 0, 2).reshape(128, 2048)
                wsh[r, goffs[gi] + u, :, 2048:] = wd[jsl, :]
    b_up = np.asarray(inputs["b_up"], f32)
    bup = np.empty((128, 4, NE, 16), f32)
    bup[:, :, :, :8] = b_up[:, :, 0::2].reshape(4, NE, 8, 128).transpose(3, 0, 1, 2)
    bup[:, :, :, 8:] = b_up[:, :, 1::2].reshape(4, NE, 8, 128).transpose(3, 0, 1, 2)
    b_down = np.asarray(inputs["b_down"], f32)
    bd2 = np.concatenate([b_down.transpose(1, 0, 2)] * 2, 0)
    shared = {
        "bmod": _fm(inputs["b_mod"].reshape(4, 6, 1024)).transpose(0, 1, 2, 3).reshape(128, 4, 48),
        "lng": _fm(inputs["ln_g"]), "lnb": _fm(inputs["ln_b"]),
        "wr": np.ascontiguousarray(np.asarray(inputs["w_router"], f32).reshape(4, 8, 128, 32).transpose(2, 0, 1, 3)),
        "br": np.ascontiguousarray(np.broadcast_to(np.asarray(inputs["b_router"], f32)[None], (128, 4, 32))),
        "bup": bup, "bd2": np.ascontiguousarray(bd2),
        "sinkb": np.ascontiguousarray(np.broadcast_to(np.asarray(inputs["sink"], f32)[None], (128, 2, 12))),
        "convw": _fm(inputs["conv_w"]),
        "ident": np.eye(128, dtype=f32),
    }
    rowi = (np.arange(S) // 64).astype(np.float64)
    coli = (np.arange(S) % 64).astype(np.float64)
    inv = 10000.0 ** (-np.arange(16, dtype=np.float64) / 16)
    a = np.concatenate([rowi[:, None] * inv, coli[:, None] * inv], -1)
    a = np.concatenate([a, a], -1)
    cosT = np.cos(a.astype(f32)).T.astype(f32)
    sinT = np.sin(a.astype(f32)).T.astype(f32)
    sinT[:32] *= -1.0
    shared["rope"] = np.ascontiguousarray(np.stack([np.concatenate([cosT, cosT], 0), np.concatenate([sinT, sinT], 0)], 1))
    cc = np.arange(64, dtype=np.float64)
    a64 = 2 * np.pi * np.outer(cc, cc) / 64
    cs64 = np.zeros((128, 256), f32)
    for g in range(2):
        cs64[g * 64:(g + 1) * 64, g * 64:(g + 1) * 64] = np.cos(a64)
        cs64[g * 64:(g + 1) * 64, 128 + g * 64:128 + (g + 1) * 64] = np.sin(a64)
    shared["cs64"] = cs64
    nn = np.arange(L, dtype=np.float64)
    aL = 2 * np.pi * np.outer(nn, nn) / L
    scL = 1.0 / np.sqrt(L * 64.0)
    tabc = np.stack([(np.cos(aL) * scL).astype(f32), (-np.sin(aL) * scL).astype(f32)], 0)
    shared["tabc"] = np.ascontiguousarray(tabc.reshape(2, 2, 128, 256).transpose(2, 0, 1, 3))
    jj_, qq_ = np.meshgrid(np.arange(128), np.arange(128), indexing="ij")
    mlo = (qq_ <= jj_).astype(f32)
    mhi = (jj_ <= qq_).astype(f32)
    shared["maskd"] = np.ascontiguousarray(np.stack([np.tile(mlo, (1, 3)), np.tile(mhi, (1, 3))], 1))
    selc = np.zeros((64, 32), f32)
    selc[np.arange(64), np.arange(64) % 32] = 1.0
    shared["selc"] = selc
    in_maps = []
    for b in range(8):
        tok = np.concatenate([x[b], ctx[b]], 0)
        m = dict(shared)
        m["xT"] = np.ascontiguousarray(tok.T.reshape(8, 128, T).transpose(1, 0, 2))
        m["cvec"] = np.ascontiguousarray(np.stack([c[b], c_ctx], -1).reshape(8, 128, 2).transpose(1, 0, 2))
        m["wsh"] = wsh[b].reshape(NU * 128, UW)
        in_maps.append(m)
    return in_maps


_NC_CACHE = {}


def kernel(**inputs):
    if "nc" not in _NC_CACHE:
        _NC_CACHE["nc"] = build()
    in_maps = prep_inputs(inputs)
    res = run_bass_kernel_spmd(_NC_CACHE["nc"], in_maps, core_ids=list(range(8)))
    out = np.empty((8, S, D), np.float32)
    for b in range(8):
        y = res.results[b]["y"]
        out[b] = y[:, :, :S].transpose(2, 1, 0).reshape(S, D)
    return out
```

```python
import numpy as np
import concourse.bass as bass
import concourse.mybir as mybir
from concourse.bass_utils import run_bass_kernel_spmd

F32, BF16 = mybir.dt.float32, mybir.dt.bfloat16
ALU = mybir.AluOpType
AF = mybir.ActivationFunctionType
AX = mybir.AxisListType

D, S, L, T = 1024, 2048, 256, 2304
NE = 32
ALPHA = 8 ** 0.25
LN_EPS = 1e-5
TT_LAT = [(0, 512), (512, 512), (1024, 512), (1536, 512)]
TT_CTX = [(2048, 256)]
UW = 3072
PAIRS = [(0, 3), (1, 4), (2, 5), (6, 9), (7, 10), (8, 11)]
ENGS = ("sp", "act", "dve", "pool", "pe")
MOE_DBG = [9]
RT_STEP = [99]
RT_NB = [99]


class Prog:
    def __init__(self, nc):
        self.nc = nc
        self.ops = {e: [] for e in ENGS}
        self.last_w = {}
        self.readers = {}
        self.waited = {e: {} for e in ENGS}
        self.dma_cnt = {}
        self.signal = {e: set() for e in ENGS}
        self.small = {e: set() for e in ENGS}
        self.default_small = False

    def add(self, eng, fn, reads=(), writes=(), dma=None, inc=16, sync_same=False, small=None):
        deps = []
        for t in reads:
            d = self.last_w.get(t)
            if d is not None:
                deps.append(d)
        for t in writes:
            d = self.last_w.get(t)
            if d is not None:
                deps.append(d)
            deps.extend(self.readers.get(t, ()))
        idx = len(self.ops[eng])
        if dma is not None:
            c = self.dma_cnt.get(dma, 0) + inc
            self.dma_cnt[dma] = c
            me = ("dma", dma, c)
        else:
            me = ("eng", eng, idx)
        wd = self.waited[eng]
        best = {}
        for d in deps:
            if d[0] == "eng":
                if d[1] == eng and dma is None and not sync_same and d[2] not in self.small[eng]:
                    continue
                key = ("eng", d[1])
            else:
                key = ("dma", d[1])
            val = d[2]
            if wd.get(key, -1) >= val:
                continue
            wd[key] = val
            if best.get(key, -1) < val:
                best[key] = val
            if d[0] == "eng":
                self.signal[d[1]].add(val)
        self.ops[eng].append((fn, list(best.items()), (dma, inc) if dma is not None else None))
        if (self.default_small if small is None else small) and dma is None:
            self.small[eng].add(idx)
        for t in reads:
            self.readers.setdefault(t, []).append(me)
        for t in writes:
            self.last_w[t] = me
            self.readers[t] = []
        return me

    def coalesce(self, sem):
        tot = self.dma_cnt.get(sem, 0)
        for k in list(self.last_w):
            d = self.last_w[k]
            if d[0] == "dma" and d[1] == sem:
                self.last_w[k] = ("dma", sem, tot)

    def barrier(self):
        lasts = [("eng", e, len(self.ops[e]) - 1) for e in ENGS if self.ops[e]]
        for e in ENGS:
            wd = self.waited[e]
            best = {}
            for d in lasts:
                if d[1] == e:
                    continue
                key = ("eng", d[1])
                if wd.get(key, -1) >= d[2]:
                    continue
                wd[key] = d[2]
                best[key] = d[2]
                self.signal[d[1]].add(d[2])
            if best:
                self.ops[e].append((None, list(best.items()), None))

    def emit(self, sems, dma_sems):
        nc = self.nc
        cum = {}
        for e in ENGS:
            cum[e] = {idx: i + 1 for i, idx in enumerate(sorted(self.signal[e]))}

        def run(e, obj):
            sig = cum[e]
            for idx, (fn, waits, dma) in enumerate(self.ops[e]):
                for (kind, key), val in waits:
                    if kind == "eng":
                        obj.wait_ge(sems[key], cum[key][val])
                    else:
                        obj.wait_ge(dma_sems[key], val)
                if fn is None:
                    if idx in sig:
                        obj.nop().then_inc(sems[e], 1)
                    continue
                inst = fn(obj)
                if dma is not None:
                    inst.then_inc(dma_sems[dma[0]], dma[1])
                    if idx in sig:
                        obj.nop().then_inc(sems[e], 1)
                elif idx in sig:
                    inst.then_inc(sems[e], 1)

        with nc.Block() as block:
            @block.sync
            def _(o):
                run("sp", o)

            @block.scalar
            def _(o):
                run("act", o)

            @block.vector
            def _(o):
                run("dve", o)

            @block.gpsimd
            def _(o):
                run("pool", o)

            @block.tensor
            def _(o):
                run("pe", o)


def unit_plan(layers):
    groups = [("misc", 6), ("tab", 4), ("mod", 8)]
    for l in layers:
        for q in range(4):
            groups.append((("moe", l, q), 8))
    offs = []
    o = 0
    for _, n in groups:
        offs.append(o)
        o += n
    return groups, offs, o


def build(layers=(0, 1, 2, 3), stop=None, n_experts=NE, single=False):
    nc = bass.Bass("TRN2", target_bir_lowering=False)
    groups, goffs, NU = unit_plan(layers)
    gidx = {g[0]: i for i, g in enumerate(groups)}

    def din(name, shape, dt=F32):
        return nc.dram_tensor(name, list(shape), dt, kind="ExternalInput")

    xT_d = din("xT", [128, 8, T])
    wsh_d = din("wsh", [NU * 128, UW])
    cvec_d = din("cvec", [128, 8, 2])
    bmod_d = din("bmod", [128, 4, 48])
    lng_d = din("lng", [128, 4, 2, 8])
    lnb_d = din("lnb", [128, 4, 2, 8])
    wr_d = din("wr", [128, 4, 8, 32])
    br_d = din("br", [128, 4, 32])
    bup_d = din("bup", [128, 4, 32, 16])
    bd2_d = din("bd2", [64, 4, 1024])
    sink_d = din("sinkb", [128, 2, 12])
    convw_d = din("convw", [128, 2, 3, 8])
    rope_d = din("rope", [128, 2, S])
    cs64_d = din("cs64", [128, 256])
    tabc_d = din("tabc", [128, 2, 2, 256])
    mask_d = din("maskd", [128, 2, 384])
    ident_d = din("ident", [128, 128])
    selc_d = din("selc", [64, 32])
    y_d = nc.dram_tensor("y", [128, 8, T], F32, kind="ExternalOutput")
    wb_d = nc.dram_tensor("wb", [NU * 128, UW], BF16)
    wg_d = [nc.dram_tensor("wg%d" % i, [8 * n * 128, UW], BF16) for i, (_, n) in enumerate(groups)]

    def wgv(gi):
        n = groups[gi][1]
        return wg_d[gi].ap().rearrange("(r u p) c -> r u p c", r=8, u=n)

    A = nc.alloc_sbuf_tensor
    XT = A("XT", [128, 8, T], F32)
    UT = A("UT", [128, 8, T], BF16)
    AT = A("AT", [128, 8 * T], BF16)
    RING = A("RING", [128, 8192], BF16)
    WD = A("WD", [128, 8192], BF16)
    SCR = A("SCR", [128, 9216], BF16)
    MODS = A("MODS", [128, 4, 48, 2], F32)
    NCS = 80
    CS = A("CS", [128, NCS, 8], F32)
    LNG = A("LNG", [128, 4, 2, 8], F32)
    LNB = A("LNB", [128, 4, 2, 8], F32)
    BUP = A("BUP", [128, 32, 16], F32)
    WR = A("WR", [128, 8, 32], F32)
    BR = A("BR", [128, 32], F32)
    IDENT = A("IDENT", [128, 128], F32)
    ONESF = A("ONESF", [128, 128], F32)
    ONESB = A("ONESB", [128, 128], BF16)
    SELC = A("SELC", [64, 32], BF16)
    SEXP = A("SEXP", [128, 2, 12], F32)
    CW = A("CW", [128, 2, 3, 8], F32)
    CVEC = A("CVEC", [128, 8, 2], F32)
    SCB = A("SCB", [128, 8, 2], BF16)
    CS64 = A("CS64", [128, 256], BF16)
    MASK = A("MASK", [128, 768], BF16)
    GHL = SCR[0:64, 6144:6144 + T]
    TABC = SCR[:, 4608:4608 + 1024]
    BM = SCR[:, 0:384].bitcast(F32).rearrange("p (l o) -> p l o", o=48)
    SMALLT = A("SMALLT", [128, 256], F32)
    SELE = A("SELE", [64, 256], BF16)
    GHBT = A("GHBT", [128, 32], BF16)
    SMALL = SMALLT[:]
    PS = [nc.alloc_psum_tensor("ps%d" % i, [128, 512], F32) for i in range(8)]

    p = Prog(nc)
    cs_names = {}

    def csi(name):
        if name not in cs_names:
            assert len(cs_names) < NCS, "CS overflow"
            cs_names[name] = len(cs_names)
        return cs_names[name]

    def csap(name, kc):
        i = csi(name)
        return CS[:, i, kc:kc + 1]

    def mm(out, lhsT, rhs, start, stop, reads, writes):
        p.add("pe", lambda e: e.matmul(out, lhsT, rhs, start=start, stop=stop), reads, writes)

    def sm(ap):
        return ap.free_size() < 256

    def act(out, in_, func, reads, writes, bias=0.0, scale=1.0):
        p.add("act", lambda e: e.activation(out=out, in_=in_, func=func, bias=bias, scale=scale), reads, writes,
              small=sm(out))

    def ts(out, in0, s1, s2, op0, op1, reads, writes, eng="dve"):
        if op1 is None:
            p.add(eng, lambda e: e.tensor_scalar(out=out, in0=in0, scalar1=s1, scalar2=None, op0=op0), reads, writes,
                  small=sm(out))
        else:
            p.add(eng, lambda e: e.tensor_scalar(out=out, in0=in0, scalar1=s1, scalar2=s2, op0=op0, op1=op1), reads, writes,
                  small=sm(out))

    def tt(out, in0, in1, op, reads, writes, eng="dve"):
        p.add(eng, lambda e: e.tensor_tensor(out=out, in0=in0, in1=in1, op=op), reads, writes, small=sm(out))

    def stt(out, in0, scalar, in1, op0, op1, reads, writes, eng="dve"):
        p.add(eng, lambda e: e.scalar_tensor_tensor(out=out, in0=in0, scalar=scalar, in1=in1, op0=op0, op1=op1),
              reads, writes, small=sm(out))

    def cp(out, in_, reads, writes, eng="dve"):
        p.add(eng, lambda e: e.tensor_copy(out=out, in_=in_), reads, writes, small=sm(out))

    def dma(q, out, in_, reads, writes, sem):
        p.add(q, lambda e: e.dma_start(out=out, in_=in_), reads, writes, dma=sem)

    def scr_f32(off_bytes, ncols):
        o = off_bytes // 2
        return SCR[:, o:o + 2 * ncols].bitcast(F32)

    def scr_bf(off_bytes, ncols):
        o = off_bytes // 2
        return SCR[:, o:o + ncols]

    for nm, t_sb, t_d in (("SELC", SELC[:], selc_d.ap()), ("CS64", CS64[:], cs64_d.ap()),
                          ("TABC", TABC, tabc_d.ap().rearrange("p a b c -> p (a b c)")),
                          ("MASK", MASK[:], mask_d.ap().rearrange("p a c -> p (a c)"))):
        dma("pool", t_sb, t_d, [], [("c", nm)], "cst")
    p.coalesce("cst")
    gorder = [gidx["mod"], gidx["misc"], gidx["tab"]] + [i for i, g in enumerate(groups) if isinstance(g[0], tuple)]
    for gi in ([] if single else gorder):
        gname, n = groups[gi]
        r0, r1 = goffs[gi] * 128, (goffs[gi] + n) * 128
        p.add("pool", (lambda a, b: (lambda e: e.dma_start(out=wb_d.ap()[a:b, :], in_=wsh_d.ap()[a:b, :])))(r0, r1),
              writes=[("wb", gi)], dma="cast")
        p.add("pool", (lambda a, b, g: (lambda e: e.collective_compute(
            "AllGather", ALU.bypass, replica_groups=[list(range(8))],
            ins=[wb_d.ap()[a:b, :]], outs=[wg_d[g].ap()])))(r0, r1, gi),
            reads=[("wb", gi)], writes=[("wg", gi)], dma="cc", inc=1)

    for kc in range(8):
        dma("sp", XT[:, kc, :], xT_d.ap()[:, kc, :], [], [("XT", kc, t0) for t0, _ in TT_LAT + TT_CTX], "ld")
    for nm, t_sb, t_d in (("CVEC", CVEC[:], cvec_d.ap()), ("BM", BM, bmod_d.ap()), ("LNG", LNG[:], lng_d.ap()),
                          ("LNB", LNB[:], lnb_d.ap()), ("SEXP", SEXP[:], sink_d.ap()), ("CW", CW[:], convw_d.ap()),
                          ("IDENT", IDENT[:], ident_d.ap())):
        dma("sp", t_sb, t_d, [], [("c", nm)], "ld")
    p.coalesce("ld")

    p.add("dve", lambda e: e.memset(ONESF[:], 1.0 / D), writes=[("c", "ONESF")])
    p.add("dve", lambda e: e.memset(ONESB[:], 1.0), writes=[("c", "ONESB")])
    act(SEXP[:], SEXP[:], AF.Exp, [("c", "SEXP")], [("c", "SEXP")])
    act(SMALL[:, 0:16], CVEC[:].rearrange("p a b -> p (a b)"), AF.Sigmoid, [("c", "CVEC")], [("small", 0)])
    tt(SCB[:].rearrange("p a b -> p (a b)"), SMALL[:, 0:16], CVEC[:].rearrange("p a b -> p (a b)"), ALU.mult,
       [("small", 0), ("c", "CVEC")], [("c", "SCB")])

    gm = gidx["mod"]
    if single:
        p.add("dve", lambda e: e.memset(MODS[:], 0.5), writes=[("c", "MODS")])
    for l in ([] if single else range(4)):
        for pc in range(8):
            half, c0 = divmod(pc * 768, UW)
            buf = AT[:, (pc % 2) * 6144:(pc % 2 + 1) * 6144].rearrange("p (k c) -> p k c", c=768)
            dma("sp", buf, wgv(gm)[:, l * 2 + half, :, c0:c0 + 768].rearrange("k p c -> p k c"),
                [("wg", gm)], [("modw", pc % 2)], "mw%d" % (pc % 2))
            for cc in range(6):
                oc = pc * 6 + cc
                for kc in range(8):
                    mm(PS[0][:, oc * 2:oc * 2 + 2], buf[:, kc, cc * 128:(cc + 1) * 128], SCB[:, kc, :],
                       kc == 0, kc == 7, [("modw", pc % 2), ("c", "SCB")], [("ps", 0)])
        for s in range(2):
            tt(MODS[:, l, :, s], PS[0][:, 0:96].rearrange("p (o s) -> p o s", s=2)[:, :, s], BM[:, l, :], ALU.add,
               [("ps", 0), ("c", "BM")], [("c", "MODS")])

    def mod_ap(l, which, s):
        return MODS[:, l, which * 8:(which + 1) * 8, s]

    def cs_set(name, fn_emit):
        i = csi(name)
        fn_emit(CS[:, i, :])
        return i

    for l in range(4):
        for s in range(2):
            sfx = "%d_%d" % (l, s)
            ts(CS[:, csi("o1" + sfx), :], mod_ap(l, 1, s), 1.0, None, ALU.add, None, [("c", "MODS")], [("cs", "o1" + sfx)])
            ts(CS[:, csi("o2" + sfx), :], mod_ap(l, 4, s), 1.0, None, ALU.add, None, [("c", "MODS")], [("cs", "o2" + sfx)])
            ts(CS[:, csi("vsc" + sfx), :], CS[:, csi("o2" + sfx), :], 1.0 / ALPHA, None, ALU.mult, None,
               [("cs", "o2" + sfx)], [("cs", "vsc" + sfx)])
            tt(CS[:, csi("a2v" + sfx), :], LNG[:, l, 0, :], CS[:, csi("o2" + sfx), :], ALU.mult,
               [("c", "LNG"), ("cs", "o2" + sfx)], [("cs", "a2v" + sfx)])
            tt(CS[:, csi("b2v" + sfx), :], LNB[:, l, 0, :], CS[:, csi("o2" + sfx), :], ALU.mult,
               [("c", "LNB"), ("cs", "o2" + sfx)], [("cs", "b2v" + sfx)])
            tt(CS[:, csi("b2v" + sfx), :], CS[:, csi("b2v" + sfx), :], mod_ap(l, 3, s), ALU.add,
               [("cs", "b2v" + sfx), ("c", "MODS")], [("cs", "b2v" + sfx)])
            if l + 1 < 4:
                nsfx = "%d_%d" % (l + 1, s)
                ts(CS[:, csi("o1n" + sfx), :], mod_ap(l + 1, 1, s), 1.0, None, ALU.add, None, [("c", "MODS")],
                   [("cs", "o1n" + sfx)])
                tt(CS[:, csi("a2u" + sfx), :], LNG[:, l, 1, :], CS[:, csi("o1n" + sfx), :], ALU.mult,
                   [("c", "LNG"), ("cs", "o1n" + sfx)], [("cs", "a2u" + sfx)])
                tt(CS[:, csi("b2u" + sfx), :], LNB[:, l, 1, :], CS[:, csi("o1n" + sfx), :], ALU.mult,
                   [("c", "LNB"), ("cs", "o1n" + sfx)], [("cs", "b2u" + sfx)])
                tt(CS[:, csi("b2u" + sfx), :], CS[:, csi("b2u" + sfx), :], mod_ap(l + 1, 0, s), ALU.add,
                   [("cs", "b2u" + sfx), ("c", "MODS")], [("cs", "b2u" + sfx)])
        for k in range(2):
            nm = "%d_%d" % (l, k)
            ts(CS[:, csi("A1" + nm), :], LNG[:, l, k, :], ALPHA, None, ALU.mult, None, [("c", "LNG")], [("cs", "A1" + nm)])
            ts(CS[:, csi("B1" + nm), :], LNB[:, l, k, :], ALPHA, None, ALU.mult, None, [("c", "LNB")], [("cs", "B1" + nm)])

    def tiles_for(ctx_on):
        return TT_LAT + (TT_CTX if ctx_on else [])

    def xt_tok(kc, t0):
        return ("XT", kc, t0)

    def ut_tok(kc, t0):
        return ("UT", kc, t0)

    def stream_of(t0):
        return 1 if t0 >= S else 0

    def initial_u():
        for kc in range(8):
            for (t0, n) in TT_LAT + TT_CTX:
                s = stream_of(t0)
                sfx = "0_%d" % s
                act(UT[:, kc, t0:t0 + n], XT[:, kc, t0:t0 + n], AF.Identity,
                    [xt_tok(kc, t0), ("cs", "o1" + sfx), ("c", "MODS")], [ut_tok(kc, t0)],
                    bias=MODS[:, 0, kc:kc + 1, s], scale=csap("o1" + sfx, kc))
                ts(XT[:, kc, t0:t0 + n], XT[:, kc, t0:t0 + n], ALPHA, None, ALU.mult, None,
                   [xt_tok(kc, t0)], [xt_tok(kc, t0)])

    def layer_norm(l, k, ctx_on, final=False):
        SQ = [scr_f32(0, 512), scr_f32(2048, 512)]
        MEAN = scr_f32(4096, 512)
        VAR = scr_f32(6144, 512)
        TB = [scr_f32(8192, 512), scr_f32(10240, 512)]
        nm = "%d_%d" % (l, k)
        cnt = 0
        for (t0, n) in tiles_for(ctx_on):
            s = stream_of(t0)
            sfx = "%d_%d" % (l, s)
            for kc in range(8):
                sq = SQ[kc % 2]
                act(sq[:, 0:n], XT[:, kc, t0:t0 + n], AF.Square, [xt_tok(kc, t0)], [("sq", kc % 2)])
                mm(PS[6][:, 0:n], ONESF[:], XT[:, kc, t0:t0 + n], kc == 0, kc == 7,
                   [xt_tok(kc, t0), ("c", "ONESF")], [("ps", 6)])
                mm(PS[7][:, 0:n], ONESF[:], sq[:, 0:n], kc == 0, kc == 7, [("sq", kc % 2)], [("ps", 7)])
            act(MEAN[:, 0:n], PS[6][:, 0:n], AF.Copy, [("ps", 6)], [("mean",)])
            act(VAR[:, 0:n], PS[6][:, 0:n], AF.Square, [("ps", 6)], [("var",)])
            tt(VAR[:, 0:n], PS[7][:, 0:n], VAR[:, 0:n], ALU.subtract, [("ps", 7), ("var",)], [("var",)])
            ts(VAR[:, 0:n], VAR[:, 0:n], LN_EPS, None, ALU.add, None, [("var",)], [("var",)])
            act(VAR[:, 0:n], VAR[:, 0:n], AF.Ln, [("var",)], [("var",)])
            act(VAR[:, 0:n], VAR[:, 0:n], AF.Exp, [("var",)], [("var",)], scale=-0.5)
            for kc in range(8):
                tb = TB[cnt % 2]
                cnt += 1
                tt(tb[:, 0:n], XT[:, kc, t0:t0 + n], MEAN[:, 0:n], ALU.subtract,
                   [xt_tok(kc, t0), ("mean",)], [("tb", cnt % 2)])
                tt(tb[:, 0:n], tb[:, 0:n], VAR[:, 0:n], ALU.mult, [("tb", cnt % 2), ("var",)], [("tb", cnt % 2)])
                if final:
                    act(XT[:, kc, t0:t0 + n], tb[:, 0:n], AF.Identity, [("tb", cnt % 2), ("c", "LNG"), ("c", "LNB")],
                        [xt_tok(kc, t0)], bias=LNB[:, l, k, kc:kc + 1], scale=LNG[:, l, k, kc:kc + 1])
                    continue
                act(XT[:, kc, t0:t0 + n], tb[:, 0:n], AF.Identity, [("tb", cnt % 2), ("cs", "A1" + nm), ("cs", "B1" + nm)],
                    [xt_tok(kc, t0)], bias=csap("B1" + nm, kc), scale=csap("A1" + nm, kc))
                if k == 0:
                    an, bn = "a2v" + sfx, "b2v" + sfx
                else:
                    an, bn = "a2u" + sfx, "b2u" + sfx
                act(UT[:, kc, t0:t0 + n], tb[:, 0:n], AF.Identity, [("tb", cnt % 2), ("cs", an), ("cs", bn)],
                    [ut_tok(kc, t0)], bias=csap(bn, kc), scale=csap(an, kc))

    def resid_from_psum(ps, l, which, oc, t0, n, ps_tok):
        s = stream_of(t0)
        stt(XT[:, oc, t0:t0 + n], ps[:, 0:n], MODS[:, l, which * 8 + oc:which * 8 + oc + 1, s], XT[:, oc, t0:t0 + n],
            ALU.mult, ALU.add, [ps_tok, xt_tok(oc, t0), ("c", "MODS")], [xt_tok(oc, t0)])

    ring_state = {"n": 0}

    def moe(l, ctx_on):
        tiles = tiles_for(ctx_on)
        nblk = 18 if ctx_on else 16
        li = layers.index(l)
        dma("sp", BUP[:], bup_d.ap()[:, l, :, :], [], [("BUP",)], "ml")
        dma("sp", WR[:], wr_d.ap()[:, l, :, :], [], [("WR",)], "ml")
        dma("sp", BR[:], br_d.ap()[:, l, :], [], [("BR",)], "ml")
        BD2 = AT[0:64, 9216:9216 + 2048].bitcast(F32)
        GT2F = AT[0:64, 0:2 * T].bitcast(F32)
        dma("sp", BD2, bd2_d.ap()[:, l, :], [], [("BD2",)], "ml")
        p.coalesce("ml")
        ts(BUP[:, :, 8:16], BUP[:, :, 8:16], 1.0, None, ALU.add, None, [("BUP",)], [("BUP",)])
        if MOE_DBG[0] < 2:
            return
        VF = scr_f32(0, 1024).rearrange("p (k t) -> p k t", t=128)
        LG = SMALL[:, 0:32]
        MX = SMALL[:, 32:40]
        NEGM = SMALL[:, 40:41]
        SSUM = SMALL[:, 41:42]
        MSK = SMALL[:, 64:96]
        EX = SMALL[:, 96:128]
        GH = SMALL[:, 128:192]
        GHB = GHBT[:]
        for nb in range(min(nblk, RT_NB[0])):
            t0 = nb * 128
            s = stream_of(t0)
            sfx = "%d_%d" % (l, s)
            tile0 = (t0 // 512) * 512 if t0 < S else S
            for kc in range(8):
                act(VF[:, kc, :], XT[:, kc, t0:t0 + 128], AF.Identity, [xt_tok(kc, tile0), ("cs", "vsc" + sfx), ("c", "MODS")],
                    [("vf", kc)], bias=MODS[:, l, 24 + kc:25 + kc, s], scale=csap("vsc" + sfx, kc))
                mm(PS[5][:, 0:32], VF[:, kc, :], WR[:, kc, :], kc == 0, kc == 7, [("vf", kc), ("WR",)], [("ps", 5)])
            if RT_STEP[0] < 1:
                continue
            tt(LG, PS[5][:, 0:32], BR[:], ALU.add, [("ps", 5), ("BR",)], [("sm",)])
            if RT_STEP[0] < 2:
                continue
            p.add("dve", lambda e: e.max(out=MX, in_=LG), [("sm",)], [("sm",)], small=True)
            ts(MSK, LG, MX[:, 3:4], None, ALU.is_ge, None, [("sm",)], [("sm",)])
            ts(NEGM, MX[:, 0:1], -1.0, None, ALU.mult, None, [("sm",)], [("sm",)])
            if RT_STEP[0] < 3:
                continue
            act(EX, LG, AF.Exp, [("sm",)], [("sm",)], bias=NEGM)
            tt(EX, EX, MSK, ALU.mult, [("sm",)], [("sm",)])
            p.add("dve", lambda e: e.tensor_reduce(out=SSUM, in_=EX, axis=AX.X, op=ALU.add), [("sm",)], [("sm",)], small=True)
            p.add("dve", lambda e: e.reciprocal(out=SSUM, in_=SSUM), [("sm",)], [("sm",)], small=True)
            ts(EX, EX, SSUM, None, ALU.mult, None, [("sm",)], [("sm",)])
            if RT_STEP[0] < 4:
                continue
            cp(GHB, EX, [("sm",)], [("ghb",)])
            if RT_STEP[0] == 41:
                continue
            cp(GH[:, 0:32], GHB, [("ghb",)], [("sm",)])
            if RT_STEP[0] == 42:
                continue
            tt(GH[:, 32:64], EX, GH[:, 0:32], ALU.subtract, [("sm",)], [("sm",)])
            if RT_STEP[0] < 5:
                continue
            mm(PS[4][0:64, (nb % 4) * 128:(nb % 4) * 128 + 128], GH, IDENT[:], True, True,
               [("sm",), ("c", "IDENT")], [("ps", 4)])
            if RT_STEP[0] != 51 and RT_STEP[0] != 53:
                cp(GT2F[:, t0:t0 + 128], PS[4][0:64, (nb % 4) * 128:(nb % 4) * 128 + 128], [("ps", 4)], [("gt2f", tile0)], eng="dve")
            if RT_STEP[0] != 51 and RT_STEP[0] != 52:
                act(GHL[:, t0:t0 + 128], GT2F[:, t0:t0 + 128], AF.Copy, [("gt2f", tile0)], [("ghl", tile0)])
        if MOE_DBG[0] < 3:
            return
        for (t0, n) in tiles:
            for oc in range(8):
                pb = PS[6 + oc % 2]
                mm(pb[:, 0:n], BD2[:, oc * 128:(oc + 1) * 128], GT2F[:, t0:t0 + n], True, True,
                   [("BD2",), ("gt2f", t0)], [("ps", 6 + oc % 2)])
                resid_from_psum(pb, l, 5, oc, t0, n, ("ps", 6 + oc % 2))
        if MOE_DBG[0] < 4:
            return
        GC = [scr_f32(0, 512)] * 2
        SG = [scr_f32(2048, 512)] * 2
        T1 = [scr_f32(4096, 512)] * 2
        GS = scr_f32(6144, 512)
        TMP = [scr_f32(8192, 512), scr_f32(10240, 512)]
        ATV = AT[:].rearrange("p (j t) -> p j t", t=T)
        WDV = WD[:].rearrange("p (j c) -> p j c", c=1024)
        it = 0
        for e in range(n_experts):
            gi = gidx[("moe", l, e // 8)]
            u = e % 8
            for j in range(8):
                sl = ring_state["n"] % 4
                ring_state["n"] += 1
                RS = RING[:, sl * 2048:(sl + 1) * 2048].rearrange("p (k c) -> p k c", c=256)
                dma("sp", RING[:, sl * 2048:(sl + 1) * 2048], wgv(gi)[j, u, :, 0:2048], [("wg", gi)], [("ring", sl)],
                    "rg%d" % sl)
                if j == 3:
                    dma("sp", WDV, wgv(gi)[:, u, :, 2048:3072].rearrange("j p c -> p j c"), [("wg", gi)], [("WD",)], "wd")
                for (t0, n) in tiles:
                    b = it % 2
                    it += 1
                    pg, pl = PS[b], PS[2 + b]
                    for kc in range(8):
                        mm(pg[:, 0:n], RS[:, kc, 0:128], UT[:, kc, t0:t0 + n], kc == 0, kc == 7,
                           [("ring", sl), ut_tok(kc, t0)], [("ps", b)])
                    for kc in range(8):
                        mm(pl[:, 0:n], RS[:, kc, 128:256], UT[:, kc, t0:t0 + n], kc == 0, kc == 7,
                           [("ring", sl), ut_tok(kc, t0)], [("ps", 2 + b)])
                    ts(T1[b][:, 0:n], pl[:, 0:n], BUP[:, e, 8 + j:9 + j], -6.0, ALU.add, ALU.max,
                       [("ps", 2 + b), ("BUP",)], [("t1",)])
                    ts(GC[b][:, 0:n], pg[:, 0:n], BUP[:, e, j:j + 1], 7.0, ALU.add, ALU.min,
                       [("ps", b), ("BUP",)], [("gc",)])
                    act(SG[b][:, 0:n], GC[b][:, 0:n], AF.Sigmoid, [("gc",)], [("sg",)], scale=1.702)
                    tt(GC[b][:, 0:n], GC[b][:, 0:n], SG[b][:, 0:n], ALU.mult, [("gc",), ("sg",)], [("gc",)])
                    stt(ATV[:, j, t0:t0 + n], T1[b][:, 0:n], 8.0, GC[b][:, 0:n], ALU.min, ALU.mult,
                        [("t1",), ("gc",)], [("at", j, t0)])
            sele = SELE[:, (e % 2) * 128:(e % 2 + 1) * 128]
            cp(sele, SELC[:, e:e + 1].to_broadcast([64, 128]), [("c", "SELC")], [("sele", e % 2)])
            for (t0, n) in tiles:
                mm(PS[4][:, 0:n], sele, GHL[:, t0:t0 + n], True, True,
                   [("sele", e % 2), ("ghl", t0)], [("ps", 4)])
                act(GS[:, 0:n], PS[4][:, 0:n], AF.Copy, [("ps", 4)], [("gs",)])
                for oc in range(8):
                    b = oc % 2
                    pd = PS[6 + b]
                    for j in range(8):
                        mm(pd[:, 0:n], WDV[:, j, oc * 128:(oc + 1) * 128], ATV[:, j, t0:t0 + n], j == 0, j == 7,
                           [("WD",), ("at", j, t0)], [("ps", 6 + b)])
                    s = stream_of(t0)
                    stt(TMP[b][:, 0:n], pd[:, 0:n], MODS[:, l, 40 + oc:41 + oc, s], GS[:, 0:n], ALU.mult, ALU.mult,
                        [("ps", 6 + b), ("gs",), ("c", "MODS")], [("tmp", b)])
                    tt(XT[:, oc, t0:t0 + n], XT[:, oc, t0:t0 + n], TMP[b][:, 0:n], ALU.add,
                       [("tmp", b), xt_tok(oc, t0)], [xt_tok(oc, t0)])

    def odd_mixer(l, ctx_on):
        j = l // 2
        tiles = tiles_for(ctx_on)
        gmi = gidx["misc"]
        WDV = WD[:].rearrange("p (k c) -> p k c", c=1024)
        dma("sp", WDV, wgv(gmi)[:, 4 + j, :, 1024:2048].rearrange("k p c -> p k c"), [("wg", gmi)], [("WD",)], "wd")
        GTV = AT[:].rearrange("p (j t) -> p j t", t=T)
        Z = scr_f32(0, 2312)
        BG = scr_f32(9248, 512)
        CB = scr_f32(11296, 512)
        A1 = scr_f32(13344, 512)
        for col in (0, 2049, 2050, 2307):
            p.add("dve", (lambda c: (lambda e: e.memset(Z[:, c:c + 1], 0.0)))(col), writes=[("zpad",)])

        def zoff(t0):
            return 1 + t0 if t0 < S else 2051 + (t0 - S)

        BGALL = AT
        for fc in range(8):
            sl = fc % 2
            RS = RING[:, sl * 3072:(sl + 1) * 3072].rearrange("p (k c) -> p k c", c=384)
            for part in range(3):
                dma("sp", RS[:, :, part * 128:(part + 1) * 128],
                    wgv(gmi)[:, j, :, part * 1024 + fc * 128:part * 1024 + (fc + 1) * 128].rearrange("k p c -> p k c"),
                    [("wg", gmi)], [("ring", sl)], "rg%d" % sl)
            for ti, (t0, n) in enumerate(tiles):
                b = ti % 2
                for part, pb in ((1, PS[b]), (2, PS[2 + b])):
                    for kc in range(8):
                        mm(pb[:, 0:n], RS[:, kc, part * 128:(part + 1) * 128], UT[:, kc, t0:t0 + n], kc == 0, kc == 7,
                           [("ring", sl), ut_tok(kc, t0)], [("ps", (0 if part == 1 else 2) + b)])
                act(CB[:, 0:n], PS[b][:, 0:n], AF.Copy, [("ps", b)], [("cb",)])
                tt(Z[:, zoff(t0):zoff(t0) + n], PS[2 + b][:, 0:n], CB[:, 0:n], ALU.mult, [("ps", 2 + b), ("cb",)], [("z", t0)])
            for ti, (t0, n) in enumerate(tiles):
                b = ti % 2
                for kc in range(8):
                    mm(PS[4 + b][:, 0:n], RS[:, kc, 0:128], UT[:, kc, t0:t0 + n], kc == 0, kc == 7,
                       [("ring", sl), ut_tok(kc, t0)], [("ps", 4 + b)])
                zo = zoff(t0)
                zr = [("z", tt0) for tt0, _ in tiles] + [("zpad",)]
                ts(A1[:, 0:n], Z[:, zo:zo + n], CW[:, j, 1, fc:fc + 1], None, ALU.mult, None, zr + [("c", "CW")], [("a1",)])
                stt(A1[:, 0:n], Z[:, zo - 1:zo - 1 + n], CW[:, j, 0, fc:fc + 1], A1[:, 0:n], ALU.mult, ALU.add,
                    zr + [("a1",)], [("a1",)])
                stt(A1[:, 0:n], Z[:, zo + 1:zo + 1 + n], CW[:, j, 2, fc:fc + 1], A1[:, 0:n], ALU.mult, ALU.add,
                    zr + [("a1",)], [("a1",)])
                tt(GTV[:, fc, t0:t0 + n], PS[4 + b][:, 0:n], A1[:, 0:n], ALU.mult, [("ps", 4 + b), ("a1",)], [("gt", fc, t0)])
        p.barrier()
        for (t0, n) in tiles:
            for oc in range(8):
                b = oc % 2
                for fc in range(8):
                    mm(PS[6 + b][:, 0:n], WDV[:, fc, oc * 128:(oc + 1) * 128], GTV[:, fc, t0:t0 + n], fc == 0, fc == 7,
                       [("WD",), ("gt", fc, t0)], [("ps", 6 + b)])
                resid_from_psum(PS[6 + b], l, 2, oc, t0, n, ("ps", 6 + b))

    def even_mixer(l, ctx_out):
        j = l // 2
        gmi, gti = gidx["misc"], gidx["tab"]
        un = 2 + j
        FTv = SCR[:, 0:2 * T].rearrange("p (f t) -> p f t", t=T)
        Gv = AT[:, 0:9216].rearrange("p (n c) -> p n c", c=512)
        SETS = [AT[:, 9216:17408].rearrange("p (n c) -> p n c", c=512), WD[:, 0:8192].rearrange("p (n c) -> p n c", c=512)]
        RS0 = RING[:, 0:2048].rearrange("p (k c) -> p k c", c=256)
        tiles_f = tiles_for(ctx_out)
        dma("sp", RS0, wgv(gmi)[:, un, :, 2304:2560].rearrange("k p c -> p k c"), [("wg", gmi)], [("ring", 0)], "rg0")
        for ti, (t0, n) in enumerate(tiles_f):
            for fcn in range(2):
                b = (ti * 2 + fcn) % 2
                for kc in range(8):
                    mm(PS[b][:, 0:n], RS0[:, kc, fcn * 128:(fcn + 1) * 128], UT[:, kc, t0:t0 + n], kc == 0, kc == 7,
                       [("ring", 0), ut_tok(kc, t0)], [("ps", b)])
                act(FTv[:, fcn, t0:t0 + n], PS[b][:, 0:n], AF.Copy, [("ps", b)], [("ft", fcn, t0)])
        nblk_f = 18 if ctx_out else 16
        for nb in range(nblk_f):
            t0 = nb * 128
            tile0 = (t0 // 512) * 512 if t0 < S else S
            for fcn in range(2):
                b = (nb * 2 + fcn) % 2
                mm(PS[4 + b][:, 0:256], FTv[:, fcn, t0:t0 + 128], CS64[:], True, True,
                   [("ft", fcn, tile0), ("c", "CS64")], [("ps", 4 + b)])
                if b == 0:
                    cp(Gv[:, nb, fcn * 256:(fcn + 1) * 256], PS[4 + b][:, 0:256], [("ps", 4 + b)], [("g", nb)])
                else:
                    act(Gv[:, nb, fcn * 256:(fcn + 1) * 256], PS[4 + b][:, 0:256], AF.Copy, [("ps", 4 + b)], [("g", nb)])
        p.barrier()
        gall = [("g", nb) for nb in range(16)]
        for kt in range(8):
            k0 = kt * 256
            st = SETS[kt % 2]
            for which in range(2):
                for u2 in range(2):
                    dma("sp", st[:, :, which * 256:(which + 1) * 256].rearrange("p (r u) c -> p r u c", u=2)[:, :, u2, :],
                        wgv(gti)[:, 2 * which + u2, :, k0:k0 + 256].rearrange("r p c -> p r c"),
                        [("wg", gti)], [("tabset", kt % 2)], "tb%d" % (kt % 2))
            for fcn in range(2):
                b = (kt * 2 + fcn) % 2
                for nb in range(16):
                    mm(PS[b][:, 0:256], Gv[:, nb, fcn * 256:fcn * 256 + 128], st[:, nb, 0:256], nb == 0, False,
                       [("tabset", kt % 2)] + gall, [("ps", b)])
                    mm(PS[b][:, 0:256], Gv[:, nb, fcn * 256 + 128:fcn * 256 + 256], st[:, nb, 256:512], False, nb == 15,
                       [("tabset", kt % 2)] + gall, [("ps", b)])
                act(FTv[:, fcn, k0:k0 + 256], PS[b][:, 0:256], AF.Copy, [("ps", b)], [("ft", fcn, (k0 // 512) * 512)])
        if ctx_out:
            TABCv = TABC.rearrange("p (a b c) -> p a b c", a=2, b=2)
            for fcn in range(2):
                for nb in range(2):
                    mm(PS[2][:, 0:256], Gv[:, 16 + nb, fcn * 256:fcn * 256 + 128], TABCv[:, 0, nb, :], nb == 0, False,
                       [("c", "TABC"), ("g", 16 + nb)], [("ps", 2)])
                    mm(PS[2][:, 0:256], Gv[:, 16 + nb, fcn * 256 + 128:fcn * 256 + 256], TABCv[:, 1, nb, :], False, nb == 1,
                       [("c", "TABC"), ("g", 16 + nb)], [("ps", 2)])
                act(FTv[:, fcn, S:S + 256], PS[2][:, 0:256], AF.Copy, [("ps", 2)], [("ft", fcn, S)])
        p.barrier()
        ROPE = RING[:].bitcast(F32).rearrange("p (a t) -> p a t", t=S)
        dma("sp", ROPE, rope_d.ap(), [], [("rope",)], "rp")
        QTv = AT[:, 0:6 * T].rearrange("p (s t) -> p s t", t=T)
        KTv = AT[:, 6 * T:8 * T].rearrange("p (s t) -> p s t", t=T)
        Vv = SCR[:, 4608:9216].rearrange("p (n c) -> p n c", c=256)
        SL = [WD[:, 0:2048].rearrange("p (k c) -> p k c", c=256), WD[:, 2048:4096].rearrange("p (k c) -> p k c", c=256)]
        X1 = [WD[:, 4096:5120].bitcast(F32), WD[:, 6144:7168].bitcast(F32)]
        X2 = [WD[:, 5120:6144].bitcast(F32), WD[:, 7168:8192].bitcast(F32)]
        pieces = [("q", s) for s in range(6)] + [("k", g2) for g2 in range(2)]
        cnt = 0
        for pi, (kind, s) in enumerate(pieces):
            sl = pi % 2
            ca = (s * 128) if kind == "q" else (1536 + s * 128)
            cb_ = (768 + s * 128) if kind == "q" else (1792 + s * 128)
            for half, c0 in ((0, ca), (1, cb_)):
                dma("sp", SL[sl][:, :, half * 128:(half + 1) * 128],
                    wgv(gmi)[:, un, :, c0:c0 + 128].rearrange("k p c -> p k c"), [("wg", gmi)], [("wsl", sl)], "ws%d" % sl)
            dst = QTv if kind == "q" else KTv
            tk = "qt" if kind == "q" else "kt"
            tl = TT_LAT + (TT_CTX if (kind == "k" or ctx_out) else [])
            for (t0, n) in tl:
                b = cnt % 2
                cnt += 1
                for kc in range(8):
                    mm(PS[b][:, 0:n], SL[sl][:, kc, 0:128], UT[:, kc, t0:t0 + n], kc == 0, kc == 7,
                       [("wsl", sl), ut_tok(kc, t0)], [("ps", b)])
                if t0 >= S:
                    act(dst[:, s, t0:t0 + n], PS[b][:, 0:n], AF.Copy, [("ps", b)], [(tk, s, t0)])
                    continue
                for kc in range(8):
                    mm(PS[2 + b][:, 0:n], SL[sl][:, kc, 128:256], UT[:, kc, t0:t0 + n], kc == 0, kc == 7,
                       [("wsl", sl), ut_tok(kc, t0)], [("ps", 2 + b)])
                tt(X1[b][:, 0:n], PS[b][:, 0:n], ROPE[:, 0, t0:t0 + n], ALU.mult, [("ps", b), ("rope",)], [("x1", b)])
                tt(X2[b][:, 0:n], PS[2 + b][:, 0:n], ROPE[:, 1, t0:t0 + n], ALU.mult, [("ps", 2 + b), ("rope",)], [("x2", b)])
                tt(dst[:, s, t0:t0 + n], X1[b][:, 0:n], X2[b][:, 0:n], ALU.add, [("x1", b), ("x2", b)], [(tk, s, t0)])
        sl = len(pieces) % 2
        dma("sp", SL[sl], wgv(gmi)[:, un, :, 2048:2304].rearrange("k p c -> p k c"), [("wg", gmi)], [("wsl", sl)], "ws%d" % sl)
        for nb in range(18):
            b = nb % 2
            t0 = nb * 128
            tile0 = (t0 // 512) * 512 if t0 < S else S
            for kc in range(8):
                mm(PS[4 + b][:, 0:256], UT[:, kc, t0:t0 + 128], SL[sl][:, kc, :], kc == 0, kc == 7,
                   [("wsl", sl), ut_tok(kc, tile0)], [("ps", 4 + b)])
            if b == 0:
                cp(Vv[:, nb, :], PS[4 + b][:, 0:256], [("ps", 4 + b)], [("v", nb)])
            else:
                act(Vv[:, nb, :], PS[4 + b][:, 0:256], AF.Copy, [("ps", 4 + b)], [("v", nb)])
        p.barrier()
        PT = [WD[:, i * 384:(i + 1) * 384] for i in range(5)]
        DN = WD[:, 2048:2816].bitcast(F32)
        qblocks = list(range(16)) + ([16, 17] if ctx_out else [])
        cnt = 0
        for qb in qblocks:
            q0 = qb * 128
            qtile = (q0 // 512) * 512 if q0 < S else S
            for g in range(4):
                h0 = 64 * (g % 2)
                g2 = g // 2
                s0 = 3 * g2
                if qb < 16:
                    kbs = [kb for kb in (qb - 1, qb, qb + 1) if 0 <= kb <= 15] + [16, 17]
                else:
                    kbs = [16, 17]
                for idx, kb in enumerate(kbs):
                    k0 = kb * 128
                    ktile = (k0 // 512) * 512 if k0 < S else S
                    psS = PS[idx % 3]
                    for i in range(3):
                        mm(psS[:, i * 128:(i + 1) * 128], KTv[h0:h0 + 64, g2, k0:k0 + 128],
                           QTv[h0:h0 + 64, s0 + i, q0:q0 + 128], True, True,
                           [("kt", g2, ktile), ("qt", s0 + i, qtile)], [("ps", idx % 3)])
                    act(PT[idx], psS[:, 0:384], AF.Exp, [("ps", idx % 3)], [("pt", idx)], scale=0.125)
                    if qb < 16 and kb == qb - 1:
                        tt(PT[idx], PT[idx], MASK[:, 0:384], ALU.mult, [("pt", idx), ("c", "MASK")], [("pt", idx)])
                    elif qb < 16 and kb == qb + 1:
                        tt(PT[idx], PT[idx], MASK[:, 384:768], ALU.mult, [("pt", idx), ("c", "MASK")], [("pt", idx)])
                b = cnt % 2
                cnt += 1
                psO, psD = PS[3 + b], PS[5 + b]
                nk = len(kbs)
                for idx, kb in enumerate(kbs):
                    mm(psO[:, 0:384], Vv[:, kb, g2 * 128:(g2 + 1) * 128], PT[idx], idx == 0, idx == nk - 1,
                       [("v", kb), ("pt", idx)], [("ps", 3 + b)])
                for idx, kb in enumerate(kbs):
                    mm(psD[:, 0:384], ONESB[:], PT[idx], idx == 0, idx == nk - 1,
                       [("c", "ONESB"), ("pt", idx)], [("ps", 5 + b)])
                for i in range(3):
                    ts(DN[h0:h0 + 64, i * 128:(i + 1) * 128], psD[h0:h0 + 64, i * 128:(i + 1) * 128],
                       SEXP[h0:h0 + 64, j, 3 * g + i:3 * g + i + 1], None, ALU.add, None,
                       [("ps", 5 + b), ("c", "SEXP")], [("dn",)])
                p.add("dve", (lambda a: (lambda e: e.reciprocal(out=DN[a:a + 64, :], in_=DN[a:a + 64, :])))(h0),
                      [("dn",)], [("dn",)], small=True)
                tt(UT[h0:h0 + 64, s0:s0 + 3, q0:q0 + 128], psO[h0:h0 + 64, 0:384].rearrange("p (s q) -> p s q", q=128),
                   DN[h0:h0 + 64, :].rearrange("p (s q) -> p s q", q=128), ALU.mult,
                   [("ps", 3 + b), ("dn",)], [ut_tok(s0 + i, qtile) for i in range(3)])
        p.barrier()
        WOV = WD[:].rearrange("p (k c) -> p k c", c=1024)
        dma("sp", WOV, wgv(gmi)[:, 4 + j, :, 0:1024].rearrange("k p c -> p k c"), [("wg", gmi)], [("WD",)], "wd")
        for (t0, n) in tiles_f:
            for oc in range(8):
                b = oc % 2
                for c8 in range(8):
                    rhs = UT[:, c8, t0:t0 + n] if c8 < 6 else FTv[:, c8 - 6, t0:t0 + n]
                    tok = ut_tok(c8, t0) if c8 < 6 else ("ft", c8 - 6, t0)
                    mm(PS[6 + b][:, 0:n], WOV[:, c8, oc * 128:(oc + 1) * 128], rhs, c8 == 0, c8 == 7,
                       [("WD",), tok], [("ps", 6 + b)])
                resid_from_psum(PS[6 + b], l, 2, oc, t0, n, ("ps", 6 + b))

    p.barrier()
    if single:
        moe(0, True)
        p.barrier()
        layers = ()
    done = False
    if 0 in layers:
        initial_u()
    p.barrier()
    for l in layers:
        even = (l % 2 == 0)
        ctx_after = (l < 2)
        mix_ctx = ctx_after
        if stop == ("pre", l):
            break
        if even:
            even_mixer(l, mix_ctx)
        else:
            odd_mixer(l, mix_ctx)
        p.barrier()
        if stop == ("mix", l):
            break
        layer_norm(l, 0, ctx_after)
        p.barrier()
        if stop == ("ln1", l):
            break
        moe(l, ctx_after)
        p.barrier()
        if stop == ("moe", l):
            break
        layer_norm(l, 1, ctx_after, final=(l == 3))
        p.barrier()
    for kc in range(8):
        dma("sp", y_d.ap()[:, kc, :], XT[:, kc, :], [xt_tok(kc, t0) for t0, _ in TT_LAT + TT_CTX], [("y", kc)], "out")
    p.add("sp", None, reads=[("y", kc) for kc in range(8)])
    sems = {e: nc.alloc_semaphore("s_" + e) for e in ENGS}
    dsems = {k: nc.alloc_semaphore("d_" + str(k)) for k in p.dma_cnt}
    with nc.allow_low_precision("bf16 matmul operands, fp32 accumulation"):
        p.emit(sems, dsems)
    return nc


def _fm(v):
    v = np.asarray(v, np.float32)
    lead = v.shape[:-1]
    return np.ascontiguousarray(np.moveaxis(v.reshape(lead + (8, 128)), -1, 0))


def prep_inputs(inputs, layers=(0, 1, 2, 3)):
    f32 = np.float32
    x, c, ctx, c_ctx = (np.asarray(inputs[k], f32) for k in ("x", "c", "ctx", "c_ctx"))
    groups, goffs, NU = unit_plan(layers)
    w_in_even, w_out_even = np.asarray(inputs["w_in_even"], f32), np.asarray(inputs["w_out_even"], f32)
    w_in_odd, w_out_odd = np.asarray(inputs["w_in_odd"], f32), np.asarray(inputs["w_out_odd"], f32)
    w_mod, w_up, w_down = inputs["w_mod"], inputs["w_up"], inputs["w_down"]
    qcols, qswap = [], []
    for lo, hi in PAIRS:
        for h in (lo, hi):
            qcols += list(range(h * 64, h * 64 + 64))
            qswap += list(range(h * 64 + 32, h * 64 + 64)) + list(range(h * 64, h * 64 + 32))
    kcols = list(range(768, 1024))
    kswap = []
    for g in range(4):
        kswap += list(range(768 + g * 64 + 32, 768 + g * 64 + 64)) + list(range(768 + g * 64, 768 + g * 64 + 32))
    ecols = qcols + qswap + kcols + kswap + list(range(1024, 1536))
    orow = qcols + list(range(768, 1024))
    n = np.arange(S, dtype=np.float64)
    ang = 2 * np.pi * np.outer(n, n) / S
    sc = 1.0 / np.sqrt(S * 64.0)
    CN = (np.cos(ang) * sc).astype(f32)
    SN = (-np.sin(ang) * sc).astype(f32)
    wsh = np.zeros((8, NU, 128, UW), f32)
    for r in range(8):
        rows = slice(r * 128, (r + 1) * 128)
        for jj in range(2):
            wsh[r, jj] = w_in_odd[jj][rows, :]
            wsh[r, 2 + jj, :, :2560] = w_in_even[jj][rows][:, ecols]
            wsh[r, 4 + jj, :, :1024] = w_out_even[jj][orow][rows, :]
            wsh[r, 4 + jj, :, 1024:2048] = w_out_odd[jj][rows, :]
        for u in range(2):
            rr = slice(r * 256 + u * 128, r * 256 + (u + 1) * 128)
            wsh[r, 6 + u, :, :S] = CN[rr]
            wsh[r, 8 + u, :, :S] = SN[rr]
        for l in range(4):
            for half in range(2):
                wsh[r, 10 + l * 2 + half] = w_mod[l][rows, half * UW:(half + 1) * UW]
    for gi, (gname, nun) in enumerate(groups):
        if not isinstance(gname, tuple):
            continue
        _, l, q = gname
        for u in range(8):
            e = q * 8 + u
            wu = np.asarray(w_up[l, e], f32)
            wd = np.asarray(w_down[l, e], f32)
            for r in range(8):
                jsl = slice(r * 128, (r + 1) * 128)
                piece = np.empty((8, 128, 256), f32)
                piece[:, :, :128] = wu[:, 0::2][:, jsl].reshape(8, 128, 128)
                piece[:, :, 128:] = wu[:, 1::2][:, jsl].reshape(8, 128, 128)
                wsh[r, goffs[gi] + u, :, :2048] = piece.transpose(1, 0, 2).reshape(128, 2048)
                wsh[r, goffs[gi] + u, :, 2048:] = wd[jsl, :]
    b_up = np.asarray(inputs["b_up"], f32)
    bup = np.empty((128, 4, NE, 16), f32)
    bup[:, :, :, :8] = b_up[:, :, 0::2].reshape(4, NE, 8, 128).transpose(3, 0, 1, 2)
    bup[:, :, :, 8:] = b_up[:, :, 1::2].reshape(4, NE, 8, 128).transpose(3, 0, 1, 2)
    b_down = np.asarray(inputs["b_down"], f32)
    bd2 = np.concatenate([b_down.transpose(1, 0, 2)] * 2, 0)
    shared = {
        "bmod": _fm(inputs["b_mod"].reshape(4, 6, 1024)).transpose(0, 1, 2, 3).reshape(128, 4, 48),
        "lng": _fm(inputs["ln_g"]), "lnb": _fm(inputs["ln_b"]),
        "wr": np.ascontiguousarray(np.asarray(inputs["w_router"], f32).reshape(4, 8, 128, 32).transpose(2, 0, 1, 3)),
        "br": np.ascontiguousarray(np.broadcast_to(np.asarray(inputs["b_router"], f32)[None], (128, 4, 32))),
        "bup": bup, "bd2": np.ascontiguousarray(bd2),
        "sinkb": np.ascontiguousarray(np.broadcast_to(np.asarray(inputs["sink"], f32)[None], (128, 2, 12))),
        "convw": _fm(inputs["conv_w"]),
        "ident": np.eye(128, dtype=f32),
    }
    rowi = (np.arange(S) // 64).astype(np.float64)
    coli = (np.arange(S) % 64).astype(np.float64)
    inv = 10000.0 ** (-np.arange(16, dtype=np.float64) / 16)
    a = np.concatenate([rowi[:, None] * inv, coli[:, None] * inv], -1)
    a = np.concatenate([a, a], -1)
    cosT = np.cos(a.astype(f32)).T.astype(f32)
    sinT = np.sin(a.astype(f32)).T.astype(f32)
    sinT[:32] *= -1.0
    shared["rope"] = np.ascontiguousarray(np.stack([np.concatenate([cosT, cosT], 0), np.concatenate([sinT, sinT], 0)], 1))
    cc = np.arange(64, dtype=np.float64)
    a64 = 2 * np.pi * np.outer(cc, cc) / 64
    cs64 = np.zeros((128, 256), f32)
    for g in range(2):
        cs64[g * 64:(g + 1) * 64, g * 64:(g + 1) * 64] = np.cos(a64)
        cs64[g * 64:(g + 1) * 64, 128 + g * 64:128 + (g + 1) * 64] = np.sin(a64)
    shared["cs64"] = cs64
    nn = np.arange(L, dtype=np.float64)
    aL = 2 * np.pi * np.outer(nn, nn) / L
    scL = 1.0 / np.sqrt(L * 64.0)
    tabc = np.stack([(np.cos(aL) * scL).astype(f32), (-np.sin(aL) * scL).astype(f32)], 0)
    shared["tabc"] = np.ascontiguousarray(tabc.reshape(2, 2, 128, 256).transpose(2, 0, 1, 3))
    jj_, qq_ = np.meshgrid(np.arange(128), np.arange(128), indexing="ij")
    mlo = (qq_ <= jj_).astype(f32)
    mhi = (jj_ <= qq_).astype(f32)
    shared["maskd"] = np.ascontiguousarray(np.stack([np.tile(mlo, (1, 3)), np.tile(mhi, (1, 3))], 1))
    selc = np.zeros((64, 32), f32)
    selc[np.arange(64), np.arange(64) % 32] = 1.0
    shared["selc"] = selc
    in_maps = []
    for b in range(8):
        tok = np.concatenate([x[b], ctx[b]], 0)
        m = dict(shared)
        m["xT"] = np.ascontiguousarray(tok.T.reshape(8, 128, T).transpose(1, 0, 2))
        m["cvec"] = np.ascontiguousarray(np.stack([c[b], c_ctx], -1).reshape(8, 128, 2).transpose(1, 0, 2))
        m["wsh"] = wsh[b].reshape(NU * 128, UW)
        in_maps.append(m)
    return in_maps


_NC_CACHE = {}


def kernel(**inputs):
    if "nc" not in _NC_CACHE:
        _NC_CACHE["nc"] = build()
    in_maps = prep_inputs(inputs)
    res = run_bass_kernel_spmd(_NC_CACHE["nc"], in_maps, core_ids=list(range(8)))
    out = np.empty((8, S, D), np.float32)
    for b in range(8):
        y = res.results[b]["y"]
        out[b] = y[:, :, :S].transpose(2, 1, 0).reshape(S, D)
    return out
```
